# Optimizing a Trainium2 kernel written in Bass

```python
import jax, jax.numpy as jnp
from jax import lax
import numpy as np

D_MODEL = 1024
BATCH = 2
SEQ = 8192
DEPTH = 4

HEAD_DIM = 64
A_HEADS = 8
B_HEADS = 8
M_HEADS = 4
M_HEAD_DIM = 128
BRANCH_WIDTH = 512
N_BRANCH = 3
MOBA_BLOCK = 256
MOBA_TOPK = 3
IDX_HEADS = 8
IDX_DIM = 64
IDX_TOPK_MAX = 256
N_MEM = 256
ROPE_THETA = 500000.0
ROT_FRACTION = 4
Q_BLOCK = 128
RMS_EPS = 1e-6
IN_WIDTHS = (
    BRANCH_WIDTH, BRANCH_WIDTH, BRANCH_WIDTH, BRANCH_WIDTH,
    BRANCH_WIDTH, BRANCH_WIDTH, BRANCH_WIDTH, BRANCH_WIDTH,
    IDX_HEADS * IDX_DIM, IDX_DIM, IDX_HEADS,
    BRANCH_WIDTH, BRANCH_WIDTH,
    N_BRANCH * D_MODEL,
)
IN_COLS = sum(IN_WIDTHS)

kernel_name = 'gated_parallel_moba_dsa_memory_trunk'


def rmsnorm(x, g):
    xf = x.astype(jnp.float32)
    y = xf * lax.rsqrt(jnp.mean(xf * xf, axis=-1, keepdims=True) + RMS_EPS)
    return (y * g.astype(jnp.float32)).astype(x.dtype)


def partial_rope(x, pos):
    d = x.shape[-1]
    rot = d // ROT_FRACTION
    half = rot // 2
    inv_freq = ROPE_THETA ** (-jnp.arange(half, dtype=jnp.float32) / half)
    ang = pos.astype(jnp.float32)[:, None] * inv_freq[None, :]
    cos = jnp.cos(ang)[:, None, :].astype(x.dtype)
    sin = jnp.sin(ang)[:, None, :].astype(x.dtype)
    x1 = x[..., :half]
    x2 = x[..., half:rot]
    return jnp.concatenate([x1 * cos - x2 * sin, x2 * cos + x1 * sin, x[..., rot:]], axis=-1)


def masked_softmax(s, mask):
    s = jnp.where(mask, s.astype(jnp.float32), -jnp.inf)
    return jax.nn.softmax(s, axis=-1)


def moba_attention(q, k, v):
    b, s, h, d = q.shape
    nb = -(-s // MOBA_BLOCK)
    pad = nb * MOBA_BLOCK - s
    padw = ((0, 0), (0, pad), (0, 0), (0, 0))
    kb = jnp.pad(k, padw).reshape(b, nb, MOBA_BLOCK, h, d).transpose(0, 3, 1, 2, 4)
    vb = jnp.pad(v, padw).reshape(b, nb, MOBA_BLOCK, h, d).transpose(0, 3, 1, 2, 4)
    kmean = jnp.mean(kb.astype(jnp.float32), axis=3).astype(q.dtype)
    kk = min(MOBA_TOPK, nb - 1)
    scale = d ** -0.5
    b_idx = jnp.arange(b)[:, None, None, None]
    h_idx = jnp.arange(h)[None, :, None, None]
    blk_ids = jnp.arange(nb)

    def one_block(qi):
        start = qi * Q_BLOCK
        own = start // MOBA_BLOCK
        qpos = start + jnp.arange(Q_BLOCK)
        qb = lax.dynamic_slice_in_dim(q, start, Q_BLOCK, axis=1).transpose(0, 2, 1, 3)
        k_own = lax.dynamic_index_in_dim(kb, own, axis=2, keepdims=False)
        v_own = lax.dynamic_index_in_dim(vb, own, axis=2, keepdims=False)
        kpos_own = own * MOBA_BLOCK + jnp.arange(MOBA_BLOCK)
        s_own = jnp.einsum('bhqd,bhkd->bhqk', qb, k_own) * scale
        m_own = jnp.broadcast_to(kpos_own[None, :] <= qpos[:, None], s_own.shape)
        if kk == 0:
            p = masked_softmax(s_own, m_own).astype(v.dtype)
            o = jnp.einsum('bhqk,bhkd->bhqd', p, v_own)
        else:
            gate = jnp.einsum('bhqd,bhnd->bhqn', qb, kmean).astype(jnp.float32)
            gate = jnp.where(blk_ids < own, gate, -jnp.inf)
            _, sel = lax.top_k(gate, kk)
            valid = sel < own
            kg = kb[b_idx, h_idx, sel]
            vg = vb[b_idx, h_idx, sel]
            s_past = jnp.einsum('bhqd,bhqjkd->bhqjk', qb, kg) * scale
            m_past = jnp.broadcast_to(valid[..., None], s_past.shape)
            n_past = kk * MOBA_BLOCK
            s_all = jnp.concatenate([s_past.reshape(b, h, Q_BLOCK, n_past), s_own], axis=-1)
            m_all = jnp.concatenate([m_past.reshape(b, h, Q_BLOCK, n_past), m_own], axis=-1)
            p = masked_softmax(s_all, m_all).astype(v.dtype)
            p_past = p[..., :n_past].reshape(b, h, Q_BLOCK, kk, MOBA_BLOCK)
            o = (jnp.einsum('bhqjk,bhqjkd->bhqd', p_past, vg)
                 + jnp.einsum('bhqk,bhkd->bhqd', p[..., n_past:], v_own))
        return o.transpose(0, 2, 1, 3)

    out = lax.map(one_block, jnp.arange(s // Q_BLOCK))
    return out.transpose(1, 0, 2, 3, 4).reshape(b, s, h, d)


def dsa_attention(q, k, v, qi, ki, wi):
    b, s, h, d = q.shape
    n_sel = min(IDX_TOPK_MAX, s // 4)
    scale = d ** -0.5
    kpos = jnp.arange(s)
    b_idx = jnp.arange(b)[:, None, None]

    def one_block(blk):
        start = blk * Q_BLOCK
        qpos = start + jnp.arange(Q_BLOCK)
        q_b = lax.dynamic_slice_in_dim(q, start, Q_BLOCK, axis=1)
        qi_b = lax.dynamic_slice_in_dim(qi, start, Q_BLOCK, axis=1)
        wi_b = lax.dynamic_slice_in_dim(wi, start, Q_BLOCK, axis=1)
        rel = jax.nn.relu(jnp.einsum('bqhd,bsd->bqhs', qi_b, ki))
        idx_score = jnp.einsum('bqhs,bqh->bqs', rel, wi_b).astype(jnp.float32)
        idx_score = jnp.where(kpos[None, :] <= qpos[:, None], idx_score, -jnp.inf)
        _, sel = lax.top_k(idx_score, n_sel)
        valid = sel <= qpos[None, :, None]
        kg = k[b_idx, sel]
        vg = v[b_idx, sel]
        sc = jnp.einsum('bqhd,bqnhd->bqhn', q_b, kg) * scale
        p = masked_softmax(sc, valid[:, :, None, :]).astype(v.dtype)
        return jnp.einsum('bqhn,bqnhd->bqhd', p, vg)

    out = lax.map(one_block, jnp.arange(s // Q_BLOCK))
    return out.transpose(1, 0, 2, 3, 4).reshape(b, s, h, d)


def memory_attention(qm, km, vm):
    sc = jnp.einsum('bshd,bmhd->bhsm', qm, km) * (qm.shape[-1] ** -0.5)
    p = jax.nn.softmax(sc.astype(jnp.float32), axis=-1).astype(vm.dtype)
    return jnp.einsum('bhsm,bmhd->bshd', p, vm)


def hybrid_layer(x, mem, pos, norm_g, w_in, mem_norm_g, w_mem_kv, w_branch, w_out):
    b, s, _ = x.shape
    h = rmsnorm(x, norm_g)
    proj = jnp.einsum('bsd,dc->bsc', h, w_in)
    offs = np.cumsum(IN_WIDTHS)[:-1].tolist()
    (a_q, a_k, a_v, a_g, b_q, b_k, b_v, b_g,
     i_q, i_k, i_w, m_q, m_g, mix) = jnp.split(proj, offs, axis=-1)

    qa = partial_rope(a_q.reshape(b, s, A_HEADS, HEAD_DIM), pos)
    ka = partial_rope(a_k.reshape(b, s, A_HEADS, HEAD_DIM), pos)
    va = a_v.reshape(b, s, A_HEADS, HEAD_DIM)
    ya = moba_attention(qa, ka, va).reshape(b, s, BRANCH_WIDTH) * jax.nn.silu(a_g)

    qb = partial_rope(b_q.reshape(b, s, B_HEADS, HEAD_DIM), pos)
    kb = partial_rope(b_k.reshape(b, s, B_HEADS, HEAD_DIM), pos)
    vb = b_v.reshape(b, s, B_HEADS, HEAD_DIM)
    qi = partial_rope(i_q.reshape(b, s, IDX_HEADS, IDX_DIM), pos) * (IDX_DIM ** -0.5)
    ki = partial_rope(i_k[:, :, None, :], pos)[:, :, 0, :]
    wi = i_w * (IDX_HEADS ** -0.5)
    yb = dsa_attention(qb, kb, vb, qi, ki, wi).reshape(b, s, BRANCH_WIDTH) * jax.nn.silu(b_g)

    mn = rmsnorm(mem, mem_norm_g)
    km, vm = jnp.split(jnp.einsum('bmd,dc->bmc', mn, w_mem_kv), 2, axis=-1)
    km = km.reshape(b, -1, M_HEADS, M_HEAD_DIM)
    vm = vm.reshape(b, -1, M_HEADS, M_HEAD_DIM)
    ym = memory_attention(m_q.reshape(b, s, M_HEADS, M_HEAD_DIM), km, vm)
    ym = ym.reshape(b, s, BRANCH_WIDTH) * jax.nn.silu(m_g)

    ys = jnp.stack([ya, yb, ym], axis=2)
    up = jnp.einsum('bsnw,nwd->bsnd', ys, w_branch)
    gates = jax.nn.sigmoid(mix.reshape(b, s, N_BRANCH, D_MODEL))
    merged = jnp.sum(gates * up, axis=2)
    return x + jnp.einsum('bsd,de->bse', merged, w_out)


def setup_inputs(seed: int = 0) -> dict:
    key = jax.random.key(seed)
    ks = jax.random.split(key, 9)
    f32 = jnp.float32
    x = jax.random.normal(ks[0], (BATCH, SEQ, D_MODEL), f32)
    mem = jax.random.normal(ks[1], (BATCH, N_MEM, D_MODEL), f32)
    norm_g = 1.0 + 0.02 * jax.random.normal(ks[2], (DEPTH, D_MODEL), f32)
    w_in = jax.random.normal(ks[3], (DEPTH, D_MODEL, IN_COLS), f32) * (D_MODEL ** -0.5)
    mem_norm_g = 1.0 + 0.02 * jax.random.normal(ks[4], (DEPTH, D_MODEL), f32)
    w_mem_kv = jax.random.normal(ks[5], (DEPTH, D_MODEL, 2 * BRANCH_WIDTH), f32) * (D_MODEL ** -0.5)
    w_branch = jax.random.normal(ks[6], (DEPTH, N_BRANCH, BRANCH_WIDTH, D_MODEL), f32) * (BRANCH_WIDTH ** -0.5)
    w_out = jax.random.normal(ks[7], (DEPTH, D_MODEL, D_MODEL), f32) * (D_MODEL ** -0.5)
    final_g = 1.0 + 0.02 * jax.random.normal(ks[8], (D_MODEL,), f32)
    return {'x': x, 'mem': mem, 'norm_g': norm_g, 'w_in': w_in, 'mem_norm_g': mem_norm_g,
            'w_mem_kv': w_mem_kv, 'w_branch': w_branch, 'w_out': w_out, 'final_g': final_g}


def reference(x, mem, norm_g, w_in, mem_norm_g, w_mem_kv, w_branch, w_out, final_g):
    pos = jnp.arange(x.shape[1], dtype=jnp.int32)
    for l in range(DEPTH):
        x = hybrid_layer(x, mem, pos, norm_g[l], w_in[l], mem_norm_g[l], w_mem_kv[l],
                         w_branch[l], w_out[l])
    return rmsnorm(x, final_g)
```

```python
import numpy as np, ml_dtypes, os
_STOP = os.environ.get('A_STOP', '')
class _StopEmit(Exception):
    pass
def _ck(name):
    if _STOP == name: raise _StopEmit()
import concourse.bass as bass, concourse.mybir as mybir
from concourse.bass_utils import run_bass_kernel_spmd
from contextlib import ExitStack

F32, BF16 = mybir.dt.float32, mybir.dt.bfloat16
AF = mybir.ActivationFunctionType
ALU = mybir.AluOpType
AX = mybir.AxisListType

NCORES = 8
D = 1024
SEQ = 8192
NSLOT = 16
TOK = 2048
INC = 8776
DEPTH = 4
NEG = -30000.0
NBIS = 17
NDS = 20
SEM_ROT = 20000


class Sched:
    def __init__(self, nc, es):
        self.nc = nc; self.es = es
        self.engs = {'pe': nc.tensor, 'act': nc.scalar, 'dve': nc.vector, 'pool': nc.gpsimd, 'sp': nc.sync}
        self.nsem = 0
        self.sem = {k: self._newsem() for k in self.engs}
        self.cnt = {k: 0 for k in self.engs}
        self.seen = {k: {} for k in self.engs}
        self.lastw = {}; self.readers = {}
        self.dpool = {e: {'sem': [self._newsem() for _ in range(NDS)], 'cnt': [0] * NDS, 'next': 0} for e in ('sp', 'pool', 'act')}
        self.dpool['coll'] = {'sem': [self._newsem() for _ in range(12)], 'cnt': [0] * 12, 'next': 0}
        self.nwait = 0; self.nins = 0

    def _newsem(self):
        self.nsem += 1
        return self.es.enter_context(self.nc.semaphore(f"sm{self.nsem}"))

    def _wait(self, eng, ev):
        sem, val, src = ev
        if src == 'pe' and eng == 'pe':
            return
        key = id(sem)
        if self.seen[eng].get(key, 0) >= val:
            return
        self.engs[eng].wait_ge(sem, val)
        self.seen[eng][key] = val; self.nwait += 1

    def _deps(self, eng, reads, writes):
        for b in reads:
            if b in self.lastw: self._wait(eng, self.lastw[b])
        for b in writes:
            if b in self.lastw: self._wait(eng, self.lastw[b])
            for ev in self.readers.get(b, ()): self._wait(eng, ev)

    def _record(self, ev, reads, writes):
        for b in reads: self.readers.setdefault(b, []).append(ev)
        for b in writes: self.lastw[b] = ev; self.readers[b] = []

    def op(self, eng, fn, reads=(), writes=()):
        self._deps(eng, reads, writes)
        ins = fn()
        if self.cnt[eng] >= SEM_ROT:
            self.sem[eng] = self._newsem(); self.cnt[eng] = 0
        self.cnt[eng] += 1; self.nins += 1
        ins.then_inc(self.sem[eng], 1)
        self._record((self.sem[eng], self.cnt[eng], eng), reads, writes)

    def dma(self, eng, out, in_, reads=(), writes=()):
        self._deps(eng, reads, writes)
        dp = self.dpool[eng]
        i = dp['next']; dp['next'] = (i + 1) % NDS
        if dp['cnt'][i] > 0: self._wait(eng, (dp['sem'][i], 16 * dp['cnt'][i], 'dma'))
        if dp['cnt'][i] >= 1000:
            dp['sem'][i] = self._newsem(); dp['cnt'][i] = 0
        dp['cnt'][i] += 1; self.nins += 1
        self.engs[eng].dma_start(out=out, in_=in_).then_inc(dp['sem'][i], 16)
        self._record((dp['sem'][i], 16 * dp['cnt'][i], 'dma'), reads, writes)

    def coll(self, fn, reads=(), writes=()):
        eng = 'pool'
        self._deps(eng, reads, writes)
        dp = self.dpool['coll']
        i = dp['next']; dp['next'] = (i + 1) % len(dp['sem'])
        if dp['cnt'][i] > 0: self._wait(eng, (dp['sem'][i], dp['cnt'][i], 'dma'))
        dp['cnt'][i] += 1; self.nins += 1
        fn().then_inc(dp['sem'][i])
        self._record((dp['sem'][i], dp['cnt'][i], 'dma'), reads, writes)

    def barrier(self):
        best = {}
        evs = list(self.lastw.values())
        for r in self.readers.values(): evs.extend(r)
        for e in self.engs:
            evs.append((self.sem[e], self.cnt[e], e))
        for (sem, val, src) in evs:
            k = id(sem)
            if val > 0 and (k not in best or best[k][1] < val): best[k] = (sem, val, 'x')
        for e in self.engs:
            for ev in best.values(): self._wait(e, ev)
        self.lastw = {}; self.readers = {}

    def finish(self, eng='sp'):
        for b, ev in list(self.lastw.items()): self._wait(eng, ev)


def _mk_ident(nc, S, identf, identb):
    S.op('pool', lambda: nc.gpsimd.memset(identf[:], 0.0), writes=['identf'])
    S.op('pool', lambda: nc.gpsimd.affine_select(out=identf[:], in_=identf[:], pattern=[[-1, 128]],
                                                 compare_op=ALU.not_equal, fill=1.0, base=0,
                                                 channel_multiplier=1), reads=['identf'], writes=['identf'])
    S.op('dve', lambda: nc.vector.tensor_copy(out=identb[:], in_=identf[:]), reads=['identf'], writes=['identb'])


P_GROUPS = [('aq', 0, 512, 'rope', 'qTa', 1.0), ('ak', 512, 512, 'rope', 'kTa', 1.0), ('av', 1024, 512, 'v', 'va', 1.0),
            ('ag', 1536, 512, 'silu', 0, 1.0), ('bq', 2048, 512, 'rope', 'qTb', 1.0), ('bk', 2560, 512, 'rope', 'kTb', 1.0),
            ('bv', 3072, 512, 'v', 'vb', 1.0), ('bg', 3584, 512, 'silu', 1, 1.0), ('iq', 4096, 512, 'rope', 'qiT', 0.125),
            ('ik', 4608, 72, 'ik', None, 1.0), ('mq', 4680, 512, 'mq', 'mqT', 1.0), ('mg', 5192, 512, 'silu', 2, 1.0)] + \
           [('mix%d' % i, 5704 + 512 * i, 512, 'sig', i, 1.0) for i in range(6)]


def emit_P(nc, S, es, io, pfx=''):
    sb = lambda name, shape, dt: es.enter_context(nc.sbuf_tensor(pfx + "s_" + name, shape, dt))
    identf = sb("identf", [128, 128], F32); identb = sb("identb", [128, 128], BF16)
    _mk_ident(nc, S, identf, identb)
    gbc = sb("gbc", [128, D], F32); mgbc = sb("mgbc", [128, D], F32)
    S.dma('sp', gbc[:], io['g'].partition_broadcast(128), writes=['gbc'])
    S.dma('sp', mgbc[:], io['mg'].partition_broadcast(128), writes=['mgbc'])
    cos_t = sb("cos_t", [128, NSLOT, 8], F32); sin_t = sb("sin_t", [128, NSLOT, 8], F32)
    for j in range(NSLOT):
        S.dma('sp', cos_t[:, j, :], io['cos'][j * 128:(j + 1) * 128, :], writes=['cos_t'])
        S.dma('sp', sin_t[:, j, :], io['sin'][j * 128:(j + 1) * 128, :], writes=['sin_t'])
    hT = sb("hT", [128, NSLOT, 8, 128], BF16)
    hmT = sb("hmT", [128, 2, 8, 128], BF16)
    xt = [sb(f"xt{i}", [128, D], F32) for i in range(2)]
    junkf = sb("junkf", [128, D], F32)
    hb = [sb(f"hb{i}", [128, D], BF16) for i in range(2)]
    st = sb("st", [128, 8], F32)
    psT = [es.enter_context(nc.psum_tensor(pfx + f"psT{i}", [128, 8, 128], BF16)) for i in range(4)]
    psA = [es.enter_context(nc.psum_tensor(pfx + f"psA{i}", [128, 512], F32)) for i in range(4)]

    def norm_tile(src_ap, gb, gbk, dst_ap, dstk, i, srck=None):
        b = i % 2
        S.dma('sp', xt[b][:], src_ap, reads=([srck] if srck else []), writes=[f'xt{b}'])
        S.op('act', lambda: nc.scalar.activation(out=junkf[:], in_=xt[b][:], func=AF.Square, accum_out=st[:, 0:1]),
             reads=[f'xt{b}'], writes=['junkf', 'st0'])
        S.op('dve', lambda: nc.vector.tensor_scalar(out=st[:, 1:2], in0=st[:, 0:1], scalar1=1.0 / D, scalar2=1e-6,
                                                    op0=ALU.mult, op1=ALU.add), reads=['st0'], writes=['st1'])
        S.op('act', lambda: nc.scalar.activation(out=st[:, 2:3], in_=st[:, 1:2], func=AF.Sqrt), reads=['st1'], writes=['st2'])
        S.op('dve', lambda: nc.vector.reciprocal(out=st[:, 3:4], in_=st[:, 2:3]), reads=['st2'], writes=['st3'])
        S.op('dve', lambda: nc.vector.scalar_tensor_tensor(out=hb[b][:], in0=xt[b][:], scalar=st[:, 3:4], in1=gb[:],
                                                           op0=ALU.mult, op1=ALU.mult),
             reads=[f'xt{b}', 'st3', gbk], writes=[f'hb{b}'])
        for k in range(8):
            S.op('pe', lambda k=k: nc.tensor.transpose(psT[b][:, k, :], hb[b][:, k * 128:(k + 1) * 128], identb[:]),
                 reads=[f'hb{b}', 'identb'], writes=[f'psT{b}'])
        S.op('act', lambda: nc.scalar.activation(out=dst_ap, in_=psT[b][:], func=AF.Copy), reads=[f'psT{b}'], writes=[dstk])

    for j in range(NSLOT):
        norm_tile(io['x'][j * 128:(j + 1) * 128, :], gbc, 'gbc', hT[:, j, :, :], f'hT{j}', j, io['xkey'](j) if 'xkey' in io else None)
    for m in range(2):
        norm_tile(io['mem'][m * 128:(m + 1) * 128, :], mgbc, 'mgbc', hmT[:, m, :, :], f'hmT{m}', m)

    wbuf = [sb(f"wbuf{i}", [128, 8, 512], BF16) for i in range(2)]
    wst = [sb(f"wst{i}", [128, 8, 512], F32) for i in range(2)]
    pr = [sb(f"pr{i}", [128, 512], F32) for i in range(4)]
    prb = [sb(f"prb{i}", [128, 512], BF16) for i in range(4)]
    rt = [sb(f"rt{i}", [128, 4, 8, 8], F32) for i in range(4)]
    oT = [sb(f"oT{i}", [128, 4, 128], BF16) for i in range(4)]
    oTk = [sb(f"oTk{i}", [64, 128], BF16) for i in range(4)]
    vt = [sb(f"vt{i}", [128, 8, 65], BF16) for i in range(4)]
    gt = [sb(f"gt{i}", [128, 512], BF16) for i in range(4)]
    wt = [sb(f"wt{i}", [128, 8], F32) for i in range(4)]
    for i in range(4):
        S.op('pool', lambda i=i: nc.gpsimd.memset(vt[i][:, :, 64:65], 1.0), writes=[f'vt{i}'])
    w_view = io['w_in'].rearrange("(k p) c -> p k c", p=128)
    cnt = {'ps': 0, 'it': 0}

    def rope(prt, prk, nh, j, rtb, rtk):
        v = prt.rearrange("p (h d) -> p h d", d=64)
        x1 = v[:, :, 0:8]; x2 = v[:, :, 8:16]
        cb = cos_t[:, j, :].unsqueeze(1).to_broadcast([128, nh, 8])
        sn = sin_t[:, j, :].unsqueeze(1).to_broadcast([128, nh, 8])
        for q, (a, bb) in enumerate([(x1, cb), (x2, sn), (x2, cb), (x1, sn)]):
            S.op('dve', lambda q=q, a=a, bb=bb: nc.vector.tensor_tensor(out=rtb[:, q, 0:nh, :], in0=a, in1=bb, op=ALU.mult),
                 reads=[prk, 'cos_t', 'sin_t'], writes=[rtk])
        S.op('dve', lambda: nc.vector.tensor_tensor(out=x1, in0=rtb[:, 0, 0:nh, :], in1=rtb[:, 1, 0:nh, :], op=ALU.subtract),
             reads=[rtk], writes=[prk])
        S.op('dve', lambda: nc.vector.tensor_tensor(out=x2, in0=rtb[:, 2, 0:nh, :], in1=rtb[:, 3, 0:nh, :], op=ALU.add),
             reads=[rtk], writes=[prk])

    def do_group(gidx, name, c0, n, kind, dst, scale, lhs, nslots, wv, is_mem=False):
        b = gidx % 2
        S.dma('sp', wst[b][:, :, 0:n], wv[:, :, c0:c0 + n], writes=[f'wst{b}'])
        S.op('pool', lambda: nc.gpsimd.tensor_copy(out=wbuf[b][:, :, 0:n], in_=wst[b][:, :, 0:n]), reads=[f'wst{b}'], writes=[f'wbuf{b}'])
        for j in range(nslots):
            pi = cnt['ps'] % 4; cnt['ps'] += 1
            it = cnt['it'] % 4; cnt['it'] += 1
            ps = psA[pi]; psk = f'psA{pi}'
            for k in range(8):
                S.op('pe', lambda k=k: nc.tensor.matmul(ps[:, 0:n], lhsT=lhs(j, k), rhs=wbuf[b][:, k, 0:n],
                                                        start=(k == 0), stop=(k == 7)),
                     reads=[f'wbuf{b}', f'hT{j}' if not is_mem else f'hmT{j}'], writes=[psk])
            if kind in ('rope', 'mq', 'kmT'):
                if kind == 'rope':
                    S.op('act', lambda: nc.scalar.activation(out=pr[it][:], in_=ps[:], func=AF.Copy, scale=scale),
                         reads=[psk], writes=[f'pr{it}'])
                    rope(pr[it][:], f'pr{it}', 8, j, rt[it], f'rt{it}')
                    S.op('dve', lambda: nc.vector.tensor_copy(out=prb[it][:], in_=pr[it][:]), reads=[f'pr{it}'], writes=[f'prb{it}'])
                else:
                    S.op('act', lambda: nc.scalar.activation(out=prb[it][:], in_=ps[:], func=AF.Copy), reads=[psk], writes=[f'prb{it}'])
                tb = it
                for q in range(4):
                    S.op('pe', lambda q=q: nc.tensor.transpose(psT[tb][:, q, :], prb[it][:, q * 128:(q + 1) * 128], identb[:]),
                         reads=[f'prb{it}', 'identb'], writes=[f'psT{tb}'])
                if kind == 'kmT':
                    S.op('act', lambda: nc.scalar.activation(out=io['kmT_sb'][:, :, j * 128:(j + 1) * 128], in_=psT[tb][:, 0:4, :], func=AF.Copy),
                         reads=[f'psT{tb}'], writes=['kmT_sb'])
                else:
                    S.op('act', lambda: nc.scalar.activation(out=oT[it][:], in_=psT[tb][:, 0:4, :], func=AF.Copy),
                         reads=[f'psT{tb}'], writes=[f'oT{it}'])
                    S.dma('act', io[dst][j * 128:(j + 1) * 128, :], oT[it][:].rearrange("p a b -> p (a b)"), reads=[f'oT{it}'], writes=[f'{dst}{j}'])
            elif kind == 'ik':
                S.op('act', lambda: nc.scalar.activation(out=pr[it][:, 0:72], in_=ps[:, 0:72], func=AF.Copy), reads=[psk], writes=[f'pr{it}'])
                rope(pr[it][:, 0:64], f'pr{it}', 1, j, rt[it], f'rt{it}')
                S.op('dve', lambda: nc.vector.tensor_copy(out=prb[it][:, 0:64], in_=pr[it][:, 0:64]), reads=[f'pr{it}'], writes=[f'prb{it}'])
                S.op('dve', lambda: nc.vector.tensor_scalar(out=wt[it][:], in0=pr[it][:, 64:72], scalar1=float(8 ** -0.5), scalar2=None, op0=ALU.mult),
                     reads=[f'pr{it}'], writes=[f'wt{it}'])
                S.dma('sp', io['wts'][j * 128:(j + 1) * 128, :], wt[it][:], reads=[f'wt{it}'], writes=[f'wts{j}'])
                S.op('pe', lambda: nc.tensor.transpose(psT[it][0:64, 0, :], prb[it][:, 0:64], identb[:]),
                     reads=[f'prb{it}', 'identb'], writes=[f'psT{it}'])
                S.op('act', lambda: nc.scalar.activation(out=oTk[it][:], in_=psT[it][0:64, 0, :], func=AF.Copy), reads=[f'psT{it}'], writes=[f'oTk{it}'])
                S.dma('act', io['kiT'][j * 64:(j + 1) * 64, :], oTk[it][:], reads=[f'oTk{it}'], writes=[f'kiT{j}'])
            elif kind == 'v':
                S.op('act', lambda: nc.scalar.activation(out=vt[it][:, :, 0:64], in_=ps[:].rearrange("p (h d) -> p h d", d=64), func=AF.Copy),
                     reads=[psk], writes=[f'vt{it}'])
                S.dma('act', io[dst][j * 128:(j + 1) * 128, :], vt[it][:].rearrange("p a b -> p (a b)"), reads=[f'vt{it}'], writes=[f'{dst}{j}'])
            elif kind == 'vm':
                S.op('act', lambda: nc.scalar.activation(out=io['vma_sb'][:, j, :, 0:128], in_=ps[:].rearrange("p (h d) -> p h d", d=128), func=AF.Copy),
                     reads=[psk], writes=['vma_sb'])
            elif kind in ('silu', 'sig'):
                fn = AF.Silu if kind == 'silu' else AF.Sigmoid
                S.op('act', lambda: nc.scalar.activation(out=gt[it][:], in_=ps[:], func=fn), reads=[psk], writes=[f'gt{it}'])
                dd = io['gates'] if kind == 'silu' else io['mix']
                S.dma('act', dd[j * 128:(j + 1) * 128, dst * 512:(dst + 1) * 512], gt[it][:], reads=[f'gt{it}'],
                      writes=[('gates%d_' % dst if kind == 'silu' else 'mix%d_' % dst) + str(j)])

    gi = 0
    order = [1, 5, 9, 2, 6] + [i for i in range(len(P_GROUPS)) if i not in (1, 5, 9, 2, 6)]
    for oi, gidx_ in enumerate(order):
        (name, c0, n, kind, dst, scale) = P_GROUPS[gidx_]
        do_group(gi, name, c0, n, kind, dst, scale, lambda j, k: hT[:, j, k, :], NSLOT, w_view); gi += 1
        if oi == 5 and 'after_kv' in io: io['after_kv']()
    io['kmT_sb'] = sb("kmT_sb", [128, 4, 256], BF16)
    io['vma_sb'] = sb("vma_sb", [128, 2, 4, 129], BF16)
    S.op('pool', lambda: nc.gpsimd.memset(io['vma_sb'][:, :, :, 128:129], 1.0), writes=['vma_sb'])
    wkv_view = io['wkv'].rearrange("(k p) c -> p k c", p=128)
    do_group(gi, 'km', 0, 512, 'kmT', None, 1.0, lambda j, k: hmT[:, j, k, :], 2, wkv_view, is_mem=True); gi += 1
    do_group(gi, 'vm', 512, 512, 'vm', None, 1.0, lambda j, k: hmT[:, j, k, :], 2, wkv_view, is_mem=True); gi += 1
    S.dma('sp', io['kmT'], io['kmT_sb'][:].rearrange("p a b -> p (a b)"), reads=['kmT_sb'], writes=['kmT'])
    S.dma('sp', io['vma'], io['vma_sb'][:].rearrange("p a b c -> p (a b c)"), reads=['vma_sb'], writes=['vma'])


P_IN = [('x', [TOK, D], F32), ('cos', [TOK, 8], F32), ('sin', [TOK, 8], F32), ('g', [1, D], F32), ('mg', [1, D], F32),
        ('w_in', [D, INC], F32), ('mem', [256, D], F32), ('wkv', [D, D], F32)]
P_OUT = [('qTa', [TOK, 512], BF16), ('qTb', [TOK, 512], BF16), ('qiT', [TOK, 512], BF16), ('mqT', [TOK, 512], BF16),
         ('kTa', [TOK, 512], BF16), ('kTb', [TOK, 512], BF16), ('kiT', [NSLOT * 64, 128], BF16),
         ('va', [TOK, 520], BF16), ('vb', [TOK, 520], BF16), ('gates', [TOK, 1536], BF16), ('wts', [TOK, 8], F32),
         ('mix', [TOK, 3072], BF16), ('kmT', [128, 1024], BF16), ('vma', [128, 1032], BF16)]


def build_P():
    nc = bass.Bass("TRN2", target_bir_lowering=False)
    io = {}
    for n, s, d in P_IN: io[n] = nc.dram_tensor(n, s, d, kind="ExternalInput").ap()
    for n, s, d in P_OUT: io[n] = nc.dram_tensor(n, s, d, kind="ExternalOutput").ap()
    with ExitStack() as es:
        S = Sched(nc, es)
        emit_P(nc, S, es, io)
        S.finish('sp')
    return nc


def emit_A(nc, S, es, io, final, nrun=NSLOT, pfx=''):
    sb = lambda name, shape, dt: es.enter_context(nc.sbuf_tensor(pfx + "s_" + name, shape, dt))
    identf = sb("identf", [128, 128], F32); identb = sb("identb", [128, 128], BF16)
    _mk_ident(nc, S, identf, identb)
    wb = sb("wb", [128, 12, D], BF16); wo = sb("wo", [128, 8, D], BF16)
    wbv = io['w_branch'].rearrange("(c p) d -> p c d", p=128)
    wov = io['w_out'].rearrange("(c p) d -> p c d", p=128)
    for c in range(0, 12, 4):
        S.dma('pool', wb[:, c:c + 4, :], wbv[:, c:c + 4, :], writes=['wb'])
    for c in range(0, 8, 4):
        S.dma('pool', wo[:, c:c + 4, :], wov[:, c:c + 4, :], writes=['wo'])
    kmT = sb("kmT_a", [128, 4, 256], BF16); vma = sb("vma_a", [128, 2, 4, 129], BF16)
    S.dma('sp', kmT[:].rearrange("p a b -> p (a b)"), io['kmT'], reads=['kmT'], writes=['kmT_s'])
    S.dma('sp', vma[:].rearrange("p a b c -> p (a b c)"), io['vma'], reads=['vma'], writes=['vma_s'])
    cmf = sb("cmf", [128, 512], F32); cmb = sb("cmb", [128, 512], BF16)
    S.dma('sp', cmf[:], io['cm'], writes=['cmf'])
    S.op('dve', lambda: nc.vector.tensor_copy(out=cmb[:], in_=cmf[:]), reads=['cmf'], writes=['cmb'])
    pastm = sb("pastm", [128, NSLOT, 32], F32); ispm = sb("ispm", [128, NSLOT, 32], F32); ownfix = sb("ownfix", [128, NSLOT, 32], F32)
    for t, k in ((pastm, 'pastm'), (ispm, 'ispm'), (ownfix, 'ownfix')):
        S.dma('sp', t[:].rearrange("p a b -> p (a b)"), io[k], writes=[k])
    fgbc = sb("fgbc", [128, D], F32)
    S.dma('sp', fgbc[:], io['fg'].partition_broadcast(128), writes=['fgbc'])
    halves = sb("halves", [128, NBIS], F32)
    for k in range(NBIS):
        S.op('pool', lambda k=k: nc.gpsimd.memset(halves[:, k:k + 1], float(2.0 ** -(k + 1))), writes=['halves'])

    kg = [sb(f"kg{i}", [128, 4, 512], BF16) for i in range(2)]
    vg = [sb(f"vg{i}", [128, 4, 520], BF16) for i in range(2)]
    kig = [sb(f"kig{i}", [128, 4, 128], BF16) for i in range(2)]
    R = [sb(f"R{i}", [128, 512], BF16) for i in range(3)]
    PT = [sb(f"PT{i}", [128, 512], BF16) for i in range(3)]
    I_all = sb("I_all", [128, SEQ], F32)
    selb = sb("selb", [128, SEQ], BF16)
    junk16 = sb("junk16", [128, SEQ], BF16)
    psS = [es.enter_context(nc.psum_tensor(pfx + f"psS{i}", [128, 512], F32)) for i in range(2)]
    psI = [es.enter_context(nc.psum_tensor(pfx + f"psI{i}", [128, 512], F32)) for i in range(1)]
    psOa = [es.enter_context(nc.psum_tensor(pfx + f"psOa{i}", [128, 512], F32)) for i in range(2)]
    psOb = [es.enter_context(nc.psum_tensor(pfx + f"psOb{i}", [128, 512], F32)) for i in range(2)]
    psMb = es.enter_context(nc.psum_tensor(pfx + "psMb", [128, 1024], BF16))
    psM = psI[0]

    class _GV:
        def __init__(self, name, p):
            self.name = name; self.p = p
        def __getitem__(self, gi):
            if self.name + '_g' in io:
                return io[self.name + '_g'].rearrange("(r j p) c -> j p r c", r=4, j=NSLOT, p=self.p)[gi]
            for ci, (s0, ns) in enumerate(GCHUNKS[self.name]):
                if s0 <= gi < s0 + ns:
                    return io[f'{self.name}_g{ci}'].rearrange("(r j p) c -> j p r c", r=4, j=ns, p=self.p)[gi - s0]
    kTa_v = _GV('kTa', 128); kTb_v = _GV('kTb', 128); va_v = _GV('va', 128); vb_v = _GV('vb', 128); ki_v = _GV('kiT', 64)

    ksum = sb("ksum", [128, 64, 4], F32)
    km32 = sb("km32", [128, 32, 4], F32)
    kmeanT = sb("kmeanT", [128, 4, 32], BF16)
    for gi in range(NSLOT):
        b = gi % 2
        S.dma('sp', kg[b][:], kTa_v[gi], reads=['kTa_g'], writes=[f'kg{b}'])
        S.op('dve', lambda gi=gi, b=b: nc.vector.tensor_reduce(
            out=ksum[:, gi * 4:(gi + 1) * 4, :], in_=kg[b][:].rearrange("p c (a t) -> p c a t", t=128), axis=AX.X, op=ALU.add),
            reads=[f'kg{b}'], writes=['ksum'])
    ksv = ksum[:].rearrange("p (b two) a -> p b two a", two=2)
    S.op('dve', lambda: nc.vector.tensor_tensor(out=km32[:], in0=ksv[:, :, 0, :], in1=ksv[:, :, 1, :], op=ALU.add), reads=['ksum'], writes=['km32'])
    S.op('dve', lambda: nc.vector.tensor_scalar(out=kmeanT[:].rearrange("p a b -> p b a"), in0=km32[:], scalar1=1.0 / 256, scalar2=None, op0=ALU.mult),
         reads=['km32'], writes=['kmeanT'])

    _ck('init')
    qTa = sb("qTa_t", [128, 8, 128], BF16); qTb = sb("qTb_t", [128, 8, 128], BF16)
    qiT = sb("qiT_t", [128, 8, 128], BF16); mqT = sb("mqT_t", [128, 4, 128], BF16)
    for t_, k_ in ((qTa, 'qTa'), (qTb, 'qTb'), (qiT, 'qiT')):
        S.op('pool', lambda t_=t_: nc.gpsimd.memset(t_[:], 0.0), writes=[k_ + '_ta', k_ + '_tb'])
    wts = sb("wts_t", [128, 8], F32); gA = sb("gA_t", [128, 512], BF16); gBM = sb("gBM_t", [128, 1024], BF16); mix = sb("mix_t", [128, 3072], BF16)
    xt = sb("xt_t", [128, D], F32)
    diagw = sb("diagw", [128, 8, 128], BF16)
    bs = sb("bs", [128, 16], F32)
    steps = sb("steps", [128, NBIS], F32)
    gm = sb("gm", [128, 8, 32], F32); top8 = sb("top8", [128, 8, 8], F32); gsel = sb("gsel", [128, 8, 32], F32)
    gbb = sb("gbb", [128, 8, 32], BF16)
    rec = sb("rec", [128, 8], F32); yf = sb("yf", [128, 512], F32)
    ycat = sb("ycat", [128, 3, 512], BF16)
    yT = sb("yT", [128, 12, 128], BF16)
    merged = sb("merged", [128, D], F32); mtmp = sb("mtmp", [128, 512], F32); mergb = sb("mergb", [128, D], BF16)
    mT = sb("mT", [128, 8, 128], BF16)
    xo = sb("xo", [128, D], F32)
    rot = {'s': 0, 'p': 0, 'r': 0}

    def hp(h):
        return slice((h % 2) * 64, (h % 2) * 64 + 64)

    def mk(j):
        G = j + 1
        n = 512 * G
        rows = slice(j * 128, (j + 1) * 128)

        def loads_AB():
            for t, k in ((qiT, 'qiT'), (qTa, 'qTa')):
                tv = t[:].rearrange("p (a two) t -> p a two t", two=2)
                S.dma('sp', tv[0:64, :, 0, :], io[k][j * 128:j * 128 + 64, :].rearrange("p (a t) -> p a t", t=128), reads=[f'{k}{j}'], writes=[k + '_ta'])
                S.dma('sp', tv[64:128, :, 1, :], io[k][j * 128 + 64:j * 128 + 128, :].rearrange("p (a t) -> p a t", t=128), reads=[f'{k}{j}'], writes=[k + '_tb'])
                if k == 'qiT':
                    S.dma('sp', wts[:], io['wts'][rows, :], reads=[f'wts{j}'], writes=['wts_t'])
            S.dma('sp', gA[:], io['gates'][rows, 0:512], reads=[f'gates0_{j}'], writes=['gA_t'])

        def loads_C():
            tv = qTb[:].rearrange("p (a two) t -> p a two t", two=2)
            S.dma('sp', tv[0:64, :, 0, :], io['qTb'][j * 128:j * 128 + 64, :].rearrange("p (a t) -> p a t", t=128), reads=[f'qTb{j}'], writes=['qTb_ta'])
            S.dma('sp', tv[64:128, :, 1, :], io['qTb'][j * 128 + 64:j * 128 + 128, :].rearrange("p (a t) -> p a t", t=128), reads=[f'qTb{j}'], writes=['qTb_tb'])
            S.dma('sp', mqT[:].rearrange("p a b -> p (a b)"), io['mqT'][rows, :], reads=[f'mqT{j}'], writes=['mqT_t'])
            S.dma('sp', gBM[:], io['gates'][rows, 512:1536], reads=[f'gates1_{j}', f'gates2_{j}'], writes=['gBM_t'])
            S.dma('sp', mix[:], io['mix'][rows, :], reads=[f'mix{n_}_{j}' for n_ in range(6)], writes=['mix_t'])
            S.dma('sp', xt[:], io['x'][rows, :], reads=([io['xkey'](j)] if 'xkey' in io else []), writes=['xt_t'])

        def diag():
            for h in range(8):
                S.op('dve', lambda h=h: nc.vector.tensor_scalar(out=diagw[:, h, :], in0=identf[:], scalar1=wts[:, h:h + 1], scalar2=None, op0=ALU.mult),
                     reads=['identf', 'wts_t'], writes=['diagw'])


        def idx():
            def ld_ki(gi):
                b = gi % 2
                S.dma('sp', kig[b][0:64, :, :], ki_v[gi], reads=['kiT_g'], writes=[f'kig{b}a'])
                S.dma('sp', kig[b][64:128, :, :], ki_v[gi], reads=['kiT_g'], writes=[f'kig{b}b'])
            items = [(gi, h) for gi in range(G) for h in range(8)]
            sbank = {}

            def emit_S(i):
                gi, h = items[i]
                si = rot['s'] % 2; rot['s'] += 1; sbank[i] = si
                S.op('pe', lambda: nc.tensor.matmul(psS[si][:], lhsT=qiT[:, h, :],
                                                    rhs=kig[gi % 2][:].rearrange("p a b -> p (a b)"), start=True, stop=True),
                     reads=['qiT_ta', 'qiT_tb', f'kig{gi % 2}a', f'kig{gi % 2}b'], writes=[f'psS{si}'])
            ld_ki(0)
            emit_S(0)
            for i, (gi, h) in enumerate(items):
                if h == 0 and gi + 1 < G: ld_ki(gi + 1)
                if i + 1 < len(items): emit_S(i + 1)
                si = sbank[i]; ri = rot['r'] % 3; rot['r'] += 1
                S.op('act', lambda: nc.scalar.activation(out=R[ri][:], in_=psS[si][:], func=AF.Relu), reads=[f'psS{si}'], writes=[f'R{ri}'])
                ib = 0
                S.op('pe', lambda: nc.tensor.matmul(psI[ib][:], lhsT=diagw[:, h, :], rhs=R[ri][:], start=(h == 0), stop=(h == 7)),
                     reads=['diagw', f'R{ri}'], writes=[f'psI{ib}'])
                if h == 7:
                    dst = I_all[:, gi * 512:(gi + 1) * 512]
                    if gi == j:
                        S.op('dve', lambda: nc.vector.tensor_tensor(out=dst, in0=psI[ib][:], in1=cmf[:], op=ALU.add),
                             reads=[f'psI{ib}', 'cmf'], writes=['I_all'])
                    else:
                        S.op('dve', lambda: nc.vector.tensor_copy(out=dst, in_=psI[ib][:]), reads=[f'psI{ib}'], writes=['I_all'])


        def gate():
            for h in range(8):
                S.op('pe', lambda h=h: nc.tensor.matmul(psI[0][:, h * 32:(h + 1) * 32], lhsT=qTa[:, h, :], rhs=kmeanT[:, h // 2, :],
                                                        start=(h == 0), stop=(h == 7), skip_group_check=True),
                     reads=['qTa_ta', 'qTa_tb', 'kmeanT'], writes=['psI0'])
            S.op('dve', lambda: nc.vector.tensor_tensor(out=gm[:], in0=psI[0][:, 0:256].rearrange("p (h b) -> p h b", b=32),
                                                        in1=pastm[:, j, :].unsqueeze(1).to_broadcast([128, 8, 32]), op=ALU.add),
                 reads=['psI0', 'pastm'], writes=['gm'])
            for h in range(8):
                S.op('dve', lambda h=h: nc.vector.max(out=top8[:, h, :], in_=gm[:, h, :]), reads=['gm'], writes=['top8'])
            S.op('dve', lambda: nc.vector.tensor_tensor(out=gsel[:], in0=gm[:], in1=top8[:, :, 2:3].to_broadcast([128, 8, 32]), op=ALU.is_lt),
                 reads=['gm', 'top8'], writes=['gsel'])
            S.op('dve', lambda: nc.vector.tensor_tensor(out=gsel[:], in0=gsel[:], in1=ispm[:, j, :].unsqueeze(1).to_broadcast([128, 8, 32]), op=ALU.mult),
                 reads=['gsel', 'ispm'], writes=['gsel'])
            S.op('dve', lambda: nc.vector.tensor_tensor(out=gbb[:], in0=gsel[:], in1=ownfix[:, j, :].unsqueeze(1).to_broadcast([128, 8, 32]), op=ALU.add),
                 reads=['gsel', 'ownfix'], writes=['gbb'])


        def bisect():
            Iv = I_all[:, 0:n]
            S.op('dve', lambda: nc.vector.tensor_reduce(out=bs[:, 0:1], in_=Iv, axis=AX.X, op=ALU.max), reads=['I_all'], writes=['bs0'])
            S.op('dve', lambda: nc.vector.scalar_tensor_tensor(out=mtmp[:], in0=cmf[:], scalar=-2.0, in1=I_all[:, n - 512:n], op0=ALU.mult, op1=ALU.add),
                 reads=['cmf', 'I_all'], writes=['mtmp'])
            S.op('dve', lambda: nc.vector.tensor_reduce(out=bs[:, 1:2], in_=mtmp[:], axis=AX.X, op=ALU.min), reads=['mtmp'], writes=['bs1'])
            if G > 1:
                S.op('dve', lambda: nc.vector.tensor_reduce(out=bs[:, 2:3], in_=I_all[:, 0:n - 512], axis=AX.X, op=ALU.min), reads=['I_all'], writes=['bs2'])
                S.op('dve', lambda: nc.vector.tensor_tensor(out=bs[:, 1:2], in0=bs[:, 1:2], in1=bs[:, 2:3], op=ALU.min), reads=['bs1', 'bs2'], writes=['bs1'])
            S.op('dve', lambda: nc.vector.scalar_tensor_tensor(out=bs[:, 3:4], in0=bs[:, 0:1], scalar=1.0, in1=bs[:, 1:2], op0=ALU.add, op1=ALU.subtract),
                 reads=['bs0', 'bs1'], writes=['bs3'])
            S.op('dve', lambda: nc.vector.tensor_scalar(out=steps[:], in0=halves[:], scalar1=bs[:, 3:4], scalar2=None, op0=ALU.mult),
                 reads=['halves', 'bs3'], writes=['steps'])
            for k in range(NBIS):
                S.op('dve', lambda k=k: nc.vector.tensor_tensor(out=bs[:, 4:5], in0=bs[:, 1:2], in1=steps[:, k:k + 1], op=ALU.add),
                     reads=['bs1', 'steps'], writes=['bs4'])
                S.op('dve', lambda: nc.vector.tensor_scalar(out=junk16[:, 0:n], in0=Iv, scalar1=bs[:, 4:5], scalar2=None, op0=ALU.is_ge, op1=ALU.add,
                                                            accum_out=bs[:, 5:6]), reads=['I_all', 'bs4'], writes=['junk16', 'bs5'])
                S.op('dve', lambda k=k: nc.vector.tensor_scalar(out=bs[:, 6:7], in0=bs[:, 5:6], scalar1=255.5, scalar2=steps[:, k:k + 1], op0=ALU.is_ge, op1=ALU.mult),
                     reads=['bs5', 'steps'], writes=['bs6'])
                S.op('dve', lambda: nc.vector.tensor_tensor(out=bs[:, 1:2], in0=bs[:, 1:2], in1=bs[:, 6:7], op=ALU.add), reads=['bs1', 'bs6'], writes=['bs1'])

        def bisect_final():
            Iv = I_all[:, 0:n]
            S.op('dve', lambda: nc.vector.tensor_scalar(out=selb[:, 0:n], in0=Iv, scalar1=bs[:, 1:2], scalar2=NEG, op0=ALU.is_lt, op1=ALU.mult),
                 reads=['I_all', 'bs1'], writes=['selb'])


        def attn_pass(kview, vview, qT, qk, kind, kgk, vgk, pso, psok):
            itm = [(gi, h) for gi in range(G) for h in range(8)]
            sbk = {}

            def ld(gi):
                b = gi % 2
                S.dma('sp', kg[b][:], kview[gi], reads=[kgk], writes=[f'kg{b}'])
                S.dma('sp', vg[b][:], vview[gi], reads=[vgk], writes=[f'vg{b}'])

            def emit_qk(i):
                gi, h = itm[i]
                si = rot['s'] % 2; rot['s'] += 1; sbk[i] = si
                b = gi % 2
                first = True
                for cc in range(4):
                    mms = []
                    mms.append((kg[b][:, cc, (h // 2) * 128:(h // 2) * 128 + 128], qT[:, h, :], [f'kg{b}', qk + 'a', qk + 'b']))
                    if kind == 'moba':
                        blk = 2 * gi + cc // 2
                        mms.append((gbb[:, h, blk:blk + 1].to_broadcast([128, 128]), identb[:], ['gbb', 'identb']))
                        if gi == j:
                            mms.append((cmb[:, cc * 128:(cc + 1) * 128], identb[:], ['cmb', 'identb']))
                    else:
                        c = 4 * gi + cc
                        mms.append((selb[:, c * 128:(c + 1) * 128], identb[:], ['selb', 'identb']))
                    for mi, (l, r, rd) in enumerate(mms):
                        last = (cc == 3 and mi == len(mms) - 1)
                        S.op('pe', lambda l=l, r=r, first=first, last=last: nc.tensor.matmul(
                            psS[si][:, cc * 128:(cc + 1) * 128], lhsT=l, rhs=r, start=first, stop=last, skip_group_check=True),
                            reads=rd, writes=[f'psS{si}'])
                        first = False
            ld(0)
            emit_qk(0)
            for i, (gi, h) in enumerate(itm):
                if h == 0 and gi + 1 < G: ld(gi + 1)
                if i + 1 < len(itm): emit_qk(i + 1)
                si = sbk[i]; pi = rot['p'] % 3; rot['p'] += 1
                S.op('act', lambda: nc.scalar.activation(out=PT[pi][:], in_=psS[si][:], func=AF.Exp, scale=0.125),
                     reads=[f'psS{si}'], writes=[f'PT{pi}'])
                ob = h // 4
                for cc in range(4):
                    first = (gi == 0 and cc == 0 and h % 4 == 0)
                    last = (gi == G - 1 and cc == 3 and h % 4 == 3)
                    S.op('pe', lambda cc=cc, first=first, last=last: nc.tensor.matmul(
                        pso[ob][:, (h % 4) * 65:(h % 4) * 65 + 65], lhsT=PT[pi][:, cc * 128:(cc + 1) * 128],
                        rhs=vg[gi % 2][:, cc, h * 65:(h + 1) * 65], start=first, stop=last, skip_group_check=True),
                        reads=[f'PT{pi}', f'vg{gi % 2}'], writes=[f'{psok}{ob}'])

        def finish_pass(nb, nh_per, dh, banks, bkeys, yout, youtk, gap, gak):
            for bi, (bank, bk) in enumerate(zip(banks, bkeys)):
                v = bank[:, 0:nh_per * (dh + 1)].rearrange("p (h e) -> p h e", e=dh + 1)
                S.op('dve', lambda v=v, bi=bi: nc.vector.reciprocal(out=rec[:, bi * nh_per:(bi + 1) * nh_per], in_=v[:, :, dh]),
                     reads=[bk], writes=['rec'])
                S.op('dve', lambda v=v, bi=bi: nc.vector.tensor_tensor(
                    out=yf[:, bi * nh_per * dh:(bi + 1) * nh_per * dh].rearrange("p (h d) -> p h d", d=dh), in0=v[:, :, 0:dh],
                    in1=rec[:, bi * nh_per:(bi + 1) * nh_per].unsqueeze(2).to_broadcast([128, nh_per, dh]), op=ALU.mult),
                    reads=[bk, 'rec'], writes=['yf'])
            S.op('dve', lambda: nc.vector.tensor_tensor(out=yout, in0=yf[:], in1=gap, op=ALU.mult),
                 reads=['yf', gak], writes=[youtk])


        def moba():
            attn_pass(kTa_v, va_v, qTa, 'qTa_t', 'moba', 'kTa_g', 'va_g', psOa, 'psOa')

        def fin_moba():
            finish_pass(0, 4, 64, psOa, ['psOa0', 'psOa1'], ycat[:, 0, :], 'ycat0', gA[:], 'gA_t')

        def dsa():
            attn_pass(kTb_v, vb_v, qTb, 'qTb_t', 'dsa', 'kTb_g', 'vb_g', psOb, 'psOb')

        def fin_dsa():
            finish_pass(1, 4, 64, psOb, ['psOb0', 'psOb1'], ycat[:, 1, :], 'ycat1', gBM[:, 0:512], 'gBM_t')

        def mem():
            for h in range(4):
                si = rot['s'] % 2; rot['s'] += 1
                for mc in range(2):
                    S.op('pe', lambda mc=mc: nc.tensor.matmul(psS[si][:, mc * 128:(mc + 1) * 128], lhsT=kmT[:, h, mc * 128:(mc + 1) * 128],
                                                              rhs=mqT[:, h, :], start=(mc == 0), stop=(mc == 1), skip_group_check=True),
                         reads=['kmT_s', 'mqT_t'], writes=[f'psS{si}'])
                pi = rot['p'] % 3; rot['p'] += 1
                S.op('act', lambda: nc.scalar.activation(out=PT[pi][:, 0:256], in_=psS[si][:, 0:256], func=AF.Exp, scale=float(128 ** -0.5)),
                     reads=[f'psS{si}'], writes=[f'PT{pi}'])
                ob = h // 2
                for mc in range(2):
                    S.op('pe', lambda mc=mc: nc.tensor.matmul(psOb[ob][:, (h % 2) * 129:(h % 2) * 129 + 129], lhsT=PT[pi][:, mc * 128:(mc + 1) * 128],
                                                              rhs=vma[:, mc, h, :], start=(mc == 0 and h % 2 == 0), stop=(mc == 1 and h % 2 == 1),
                                                              skip_group_check=True),
                         reads=[f'PT{pi}', 'vma_s'], writes=[f'psOb{ob}'])
            finish_pass(2, 2, 128, psOb, ['psOb0', 'psOb1'], ycat[:, 2, :], 'ycat2', gBM[:, 512:1024], 'gBM_t')


        def combine():
            ycf = ycat[:].rearrange("p a b -> p (a b)")
            for c0, c1 in ((0, 8), (8, 12)):
                for c in range(c0, c1):
                    S.op('pe', lambda c=c: nc.tensor.transpose(psMb[:, (c % 8) * 128:(c % 8) * 128 + 128], ycf[:, c * 128:(c + 1) * 128], identb[:]),
                         reads=['ycat0', 'ycat1', 'ycat2', 'identb'], writes=['psM'])
                S.op('act', lambda: nc.scalar.activation(out=yT[:, c0:c1, :].rearrange("p a b -> p (a b)"), in_=psMb[:, 0:(c1 - c0) * 128], func=AF.Copy),
                     reads=['psM'], writes=['yT'])
            for nb in range(3):
                for half in range(2):
                    si = rot['s'] % 2; rot['s'] += 1
                    for c in range(4):
                        S.op('pe', lambda c=c: nc.tensor.matmul(psS[si][:], lhsT=yT[:, nb * 4 + c, :], rhs=wb[:, nb * 4 + c, half * 512:(half + 1) * 512],
                                                                start=(c == 0), stop=(c == 3)),
                             reads=['yT', 'wb'], writes=[f'psS{si}'])
                    mg_ = mix[:, nb * 1024 + half * 512: nb * 1024 + (half + 1) * 512]
                    md = merged[:, half * 512:(half + 1) * 512]
                    if nb == 0:
                        S.op('dve', lambda: nc.vector.tensor_tensor(out=md, in0=psS[si][:], in1=mg_, op=ALU.mult),
                             reads=[f'psS{si}', 'mix_t'], writes=[f'merged{half}'])
                    else:
                        S.op('dve', lambda: nc.vector.tensor_tensor(out=mtmp[:], in0=psS[si][:], in1=mg_, op=ALU.mult),
                             reads=[f'psS{si}', 'mix_t'], writes=['mtmp'])
                        S.op('dve', lambda: nc.vector.tensor_tensor(out=md, in0=md, in1=mtmp[:], op=ALU.add),
                             reads=['mtmp', f'merged{half}'], writes=[f'merged{half}'])
            S.op('dve', lambda: nc.vector.tensor_copy(out=mergb[:], in_=merged[:]), reads=['merged0', 'merged1'], writes=['mergb'])
            for c in range(8):
                S.op('pe', lambda c=c: nc.tensor.transpose(psMb[:, c * 128:(c + 1) * 128], mergb[:, c * 128:(c + 1) * 128], identb[:]),
                     reads=['mergb', 'identb'], writes=['psM'])
            S.op('act', lambda: nc.scalar.activation(out=mT[:].rearrange("p a b -> p (a b)"), in_=psMb[:, 0:1024], func=AF.Copy),
                 reads=['psM'], writes=['mT'])
            for half in range(2):
                si = rot['s'] % 2; rot['s'] += 1
                for c in range(8):
                    S.op('pe', lambda c=c: nc.tensor.matmul(psS[si][:], lhsT=mT[:, c, :], rhs=wo[:, c, half * 512:(half + 1) * 512],
                                                            start=(c == 0), stop=(c == 7)),
                         reads=['mT', 'wo'], writes=[f'psS{si}'])
                S.op('dve', lambda: nc.vector.tensor_tensor(out=xo[:, half * 512:(half + 1) * 512], in0=psS[si][:], in1=xt[:, half * 512:(half + 1) * 512], op=ALU.add),
                     reads=[f'psS{si}', 'xt_t'], writes=[f'xo{half}'])
            if final:
                S.op('act', lambda: nc.scalar.activation(out=mergb[:], in_=xo[:], func=AF.Square, accum_out=bs[:, 8:9]),
                     reads=['xo0', 'xo1'], writes=['mergb', 'bs8'])
                S.op('dve', lambda: nc.vector.tensor_scalar(out=bs[:, 9:10], in0=bs[:, 8:9], scalar1=1.0 / D, scalar2=1e-6, op0=ALU.mult, op1=ALU.add),
                     reads=['bs8'], writes=['bs9'])
                S.op('act', lambda: nc.scalar.activation(out=bs[:, 10:11], in_=bs[:, 9:10], func=AF.Sqrt), reads=['bs9'], writes=['bs10'])
                S.op('dve', lambda: nc.vector.reciprocal(out=bs[:, 11:12], in_=bs[:, 10:11]), reads=['bs10'], writes=['bs11'])
                S.op('dve', lambda: nc.vector.scalar_tensor_tensor(out=xo[:], in0=xo[:], scalar=bs[:, 11:12], in1=fgbc[:], op0=ALU.mult, op1=ALU.mult),
                     reads=['xo0', 'xo1', 'bs11', 'fgbc'], writes=['xo0', 'xo1'])
                S.dma('pool', io['xout'][rows, :], xo[:], reads=['xo0', 'xo1'], writes=[io['xokey'](j) if 'xokey' in io else 'xout'])
            else:
                S.dma('pool', io['xout'][rows, :], xo[:], reads=['xo0', 'xo1'], writes=[io['xokey'](j) if 'xokey' in io else 'xout'])

        return dict(loads_AB=loads_AB, loads_C=loads_C, diag=diag, idx=idx, gate=gate, bisect=bisect, bisect_final=bisect_final, moba=moba, fin_moba=fin_moba,
                    dsa=dsa, fin_dsa=fin_dsa, mem=mem, combine=combine)

    parts = [mk(j) for j in range(nrun)]
    for j in range(nrun):
        p = parts[j]
        p['loads_AB'](); p['diag'](); p['idx'](); p['gate'](); p['bisect']()
        if j > 0: parts[j - 1]['dsa']()
        p['bisect_final']()
        p['moba']()
        if j > 0:
            q = parts[j - 1]; q['fin_dsa'](); q['mem'](); q['combine']()
        p['fin_moba'](); p['loads_C']()
    q = parts[nrun - 1]; q['dsa'](); q['fin_dsa'](); q['mem'](); q['combine']()


A_IN = [('x', [TOK, D], F32), ('qTa', [TOK, 512], BF16), ('qTb', [TOK, 512], BF16), ('qiT', [TOK, 512], BF16), ('mqT', [TOK, 512], BF16),
        ('wts', [TOK, 8], F32), ('gates', [TOK, 1536], BF16), ('mix', [TOK, 3072], BF16),
        ('kTa_g', [4 * TOK, 512], BF16), ('kTb_g', [4 * TOK, 512], BF16), ('kiT_g', [4 * NSLOT * 64, 128], BF16),
        ('va_g', [4 * TOK, 520], BF16), ('vb_g', [4 * TOK, 520], BF16), ('kmT', [128, 1024], BF16), ('vma', [128, 1032], BF16),
        ('w_branch', [1536, D], F32), ('w_out', [D, D], F32), ('cm', [128, 512], F32),
        ('pastm', [128, NSLOT * 32], F32), ('ispm', [128, NSLOT * 32], F32), ('ownfix', [128, NSLOT * 32], F32), ('fg', [1, D], F32)]


def build_A(final, nrun=NSLOT):
    nc = bass.Bass("TRN2", target_bir_lowering=False)
    io = {}
    for n, s, d in A_IN: io[n] = nc.dram_tensor(n, s, d, kind="ExternalInput").ap()
    io['xout'] = nc.dram_tensor('xout', [TOK, D], F32, kind="ExternalOutput").ap()
    with ExitStack() as es:
        S = Sched(nc, es)
        try:
            emit_A(nc, S, es, io, final, nrun)
        except _StopEmit:
            pass
        print('A nins', S.nins, 'nwait', S.nwait, 'nsem', S.nsem)
        S.finish('sp')
    return nc


F_IN = [('x', [TOK, D], F32), ('cos', [TOK, 8], F32), ('sin', [TOK, 8], F32), ('g_all', [DEPTH, D], F32), ('mg_all', [DEPTH, D], F32),
        ('w_in_all', [DEPTH * D, INC], F32), ('mem', [256, D], F32), ('wkv_all', [DEPTH * D, D], F32),
        ('w_branch_all', [DEPTH * 1536, D], F32), ('w_out_all', [DEPTH * D, D], F32), ('cm', [128, 512], F32),
        ('pastm', [128, NSLOT * 32], F32), ('ispm', [128, NSLOT * 32], F32), ('ownfix', [128, NSLOT * 32], F32), ('fg', [1, D], F32)]
GATHER = [('kTa', 512, 128), ('kTb', 512, 128), ('kiT', 128, 64), ('va', 520, 128), ('vb', 520, 128)]
GCHUNKS = {'kTa': [(0, 8), (8, 8)], 'kTb': [(0, 8), (8, 8)], 'kiT': [(0, 16)], 'va': [(0, 7), (7, 7), (14, 2)], 'vb': [(0, 7), (7, 7), (14, 2)]}


def build_fused(depth=DEPTH, nrun=NSLOT):
    nc = bass.Bass("TRN2", target_bir_lowering=False)
    ext = {}
    for n, s_, d in F_IN: ext[n] = nc.dram_tensor(n, s_, d, kind="ExternalInput").ap()
    ext['xout'] = nc.dram_tensor('xout', [TOK, D], F32, kind="ExternalOutput").ap()
    scr = {}
    for n, s_, d in P_OUT: scr[n] = nc.dram_tensor("scr_" + n, s_, d).ap()
    for n, w, p in GATHER:
        for ci, (s0, ns) in enumerate(GCHUNKS[n]):
            scr[f'{n}_g{ci}'] = nc.dram_tensor(f"scr_{n}_g{ci}", [4 * ns * p, w], BF16).ap()
    xs = [nc.dram_tensor(f"scr_x{i}", [TOK, D], F32).ap() for i in range(2)]
    groups = [[0, 1, 2, 3], [4, 5, 6, 7]]
    with ExitStack() as es0:
        S = Sched(nc, es0)
        for l in range(depth):
            io = dict(scr)
            for k in ('cos', 'sin', 'mem', 'cm', 'pastm', 'ispm', 'ownfix', 'fg'): io[k] = ext[k]
            io['x'] = ext['x'] if l == 0 else xs[l % 2]
            io['xkey'] = (lambda j, l=l: f'xs{l}_{j}')
            io['xokey'] = (lambda j, l=l: f'xs{l + 1}_{j}')
            io['xout'] = ext['xout'] if l == depth - 1 else xs[(l + 1) % 2]
            io['g'] = ext['g_all'][l:l + 1, :]; io['mg'] = ext['mg_all'][l:l + 1, :]
            io['w_in'] = ext['w_in_all'][l * D:(l + 1) * D, :]; io['wkv'] = ext['wkv_all'][l * D:(l + 1) * D, :]
            io['w_branch'] = ext['w_branch_all'][l * 1536:(l + 1) * 1536, :]; io['w_out'] = ext['w_out_all'][l * D:(l + 1) * D, :]

            def after_kv():
                for n, w, p in GATHER:
                    for ci, (s0, ns) in enumerate(GCHUNKS[n]):
                        S.coll(lambda n=n, ci=ci, s0=s0, ns=ns, p=p: nc.gpsimd.collective_compute(
                            "AllGather", ALU.bypass, replica_groups=groups, ins=[scr[n][s0 * p:(s0 + ns) * p, :]], outs=[scr[f'{n}_g{ci}']]),
                            reads=[f'{n}{j}' for j in range(s0, s0 + ns)], writes=[n + '_g'])
            io['after_kv'] = after_kv
            with ExitStack() as es:
                emit_P(nc, S, es, io, pfx=f"P{l}_")
                S.barrier()
            with ExitStack() as es:
                try:
                    emit_A(nc, S, es, io, l == depth - 1, nrun, pfx=f"A{l}_")
                except _StopEmit:
                    pass
                S.barrier()
        S.finish('sp')
        print('fused nins', S.nins, 'nwait', S.nwait, 'nsem', S.nsem)
    return nc


def _core_tokens(r):
    return (np.arange(NSLOT)[:, None] * 4 + r)[:, :, None] * 128 + np.arange(128)[None, None, :]


def _masks(r):
    t = np.arange(128)[:, None]; s = np.arange(512)[None, :]
    cm = np.where(s <= 128 * r + t, 0.0, NEG).astype(np.float32)
    own = 2 * np.arange(NSLOT)[:, None] + r // 2
    blk = np.arange(32)[None, :]
    pastm = np.where(blk < own, 0.0, -1e30).astype(np.float32)
    ispm = np.where(blk < own, NEG, 0.0).astype(np.float32)
    ownfix = np.where(blk <= own, 0.0, NEG).astype(np.float32)
    rep = lambda a: np.ascontiguousarray(np.broadcast_to(a.reshape(1, -1), (128, a.size)))
    return cm, rep(pastm), rep(ispm), rep(ownfix)


_CACHE = {}


def kernel(x, mem, norm_g, w_in, mem_norm_g, w_mem_kv, w_branch, w_out, final_g):
    x = np.asarray(x, np.float32); mem = np.asarray(mem, np.float32)
    if 'F' not in _CACHE:
        _CACHE['F'] = build_fused()
    ncF = _CACHE['F']
    cores = list(range(NCORES))
    pos = [_core_tokens(c % 4).reshape(-1) for c in cores]
    inv_freq = (np.float32(500000.0) ** (-np.arange(8, dtype=np.float32) / np.float32(8))).astype(np.float32)
    f32c = lambda a: np.ascontiguousarray(np.asarray(a, np.float32))
    shared = {'g_all': f32c(norm_g), 'mg_all': f32c(mem_norm_g), 'w_in_all': f32c(w_in).reshape(DEPTH * D, INC),
              'wkv_all': f32c(w_mem_kv).reshape(DEPTH * D, D), 'w_branch_all': f32c(w_branch).reshape(DEPTH * 1536, D),
              'w_out_all': f32c(w_out).reshape(DEPTH * D, D), 'fg': f32c(final_g)[None, :]}
    in_maps = []
    for c in cores:
        ang = pos[c].astype(np.float32)[:, None] * inv_freq[None, :]
        cm, pastm, ispm, ownfix = _masks(c % 4)
        m = dict(shared)
        m.update({'x': np.ascontiguousarray(x[c // 4][pos[c]]), 'cos': np.cos(ang).astype(np.float32), 'sin': np.sin(ang).astype(np.float32),
                  'mem': np.ascontiguousarray(mem[c // 4]), 'cm': cm, 'pastm': pastm, 'ispm': ispm, 'ownfix': ownfix})
        in_maps.append(m)
    res = run_bass_kernel_spmd(ncF, in_maps, core_ids=cores).results
    out = np.empty((2, SEQ, D), np.float32)
    for c in cores:
        out[c // 4][pos[c]] = np.asarray(res[c]['xout'], np.float32)
    return out
```

```python
import numpy as np, ml_dtypes, os
_STOP = os.environ.get('A_STOP', '')
class _StopEmit(Exception):
    pass
def _ck(name):
    if _STOP == name: raise _StopEmit()
import concourse.bass as bass, concourse.mybir as mybir
from concourse.bass_utils import run_bass_kernel_spmd
from contextlib import ExitStack

F32, BF16 = mybir.dt.float32, mybir.dt.bfloat16
AF = mybir.ActivationFunctionType
ALU = mybir.AluOpType
AX = mybir.AxisListType

NCORES = 8
D = 1024
SEQ = 8192
NSLOT = 16
TOK = 2048
INC = 8776
DEPTH = 4
NEG = -30000.0
NBIS = 17
NDS = 32
SEM_ROT = 20000


class Sched:
    def __init__(self, nc, es):
        self.nc = nc; self.es = es
        self.engs = {'pe': nc.tensor, 'act': nc.scalar, 'dve': nc.vector, 'pool': nc.gpsimd, 'sp': nc.sync}
        self.nsem = 0
        self.sem = {k: self._newsem() for k in self.engs}
        self.cnt = {k: 0 for k in self.engs}
        self.seen = {k: {} for k in self.engs}
        self.lastw = {}; self.readers = {}
        self.dpool = {e: {'sem': [self._newsem() for _ in range(NDS)], 'cnt': [0] * NDS, 'next': 0} for e in ('sp', 'pool')}
        self.dpool['coll'] = {'sem': [self._newsem() for _ in range(12)], 'cnt': [0] * 12, 'next': 0}
        self.nwait = 0; self.nins = 0

    def _newsem(self):
        self.nsem += 1
        return self.es.enter_context(self.nc.semaphore(f"sm{self.nsem}"))

    def _wait(self, eng, ev):
        sem, val, src = ev
        if src == 'pe' and eng == 'pe':
            return
        key = id(sem)
        if self.seen[eng].get(key, 0) >= val:
            return
        self.engs[eng].wait_ge(sem, val)
        self.seen[eng][key] = val; self.nwait += 1

    def _deps(self, eng, reads, writes):
        for b in reads:
            if b in self.lastw: self._wait(eng, self.lastw[b])
        for b in writes:
            if b in self.lastw: self._wait(eng, self.lastw[b])
            for ev in self.readers.get(b, ()): self._wait(eng, ev)

    def _record(self, ev, reads, writes):
        for b in reads: self.readers.setdefault(b, []).append(ev)
        for b in writes: self.lastw[b] = ev; self.readers[b] = []

    def op(self, eng, fn, reads=(), writes=()):
        self._deps(eng, reads, writes)
        ins = fn()
        if self.cnt[eng] >= SEM_ROT:
            self.sem[eng] = self._newsem(); self.cnt[eng] = 0
        self.cnt[eng] += 1; self.nins += 1
        ins.then_inc(self.sem[eng], 1)
        self._record((self.sem[eng], self.cnt[eng], eng), reads, writes)

    def dma(self, eng, out, in_, reads=(), writes=()):
        self._deps(eng, reads, writes)
        dp = self.dpool[eng]
        i = dp['next']; dp['next'] = (i + 1) % NDS
        if dp['cnt'][i] > 0: self._wait(eng, (dp['sem'][i], 16 * dp['cnt'][i], 'dma'))
        if dp['cnt'][i] >= 1000:
            dp['sem'][i] = self._newsem(); dp['cnt'][i] = 0
        dp['cnt'][i] += 1; self.nins += 1
        self.engs[eng].dma_start(out=out, in_=in_).then_inc(dp['sem'][i], 16)
        self._record((dp['sem'][i], 16 * dp['cnt'][i], 'dma'), reads, writes)

    def coll(self, fn, reads=(), writes=()):
        eng = 'pool'
        self._deps(eng, reads, writes)
        dp = self.dpool['coll']
        i = dp['next']; dp['next'] = (i + 1) % len(dp['sem'])
        if dp['cnt'][i] > 0: self._wait(eng, (dp['sem'][i], dp['cnt'][i], 'dma'))
        dp['cnt'][i] += 1; self.nins += 1
        fn().then_inc(dp['sem'][i])
        self._record((dp['sem'][i], dp['cnt'][i], 'dma'), reads, writes)

    def barrier(self):
        best = {}
        evs = list(self.lastw.values())
        for r in self.readers.values(): evs.extend(r)
        for e in self.engs:
            evs.append((self.sem[e], self.cnt[e], e))
        for (sem, val, src) in evs:
            k = id(sem)
            if val > 0 and (k not in best or best[k][1] < val): best[k] = (sem, val, 'x')
        for e in self.engs:
            for ev in best.values(): self._wait(e, ev)
        self.lastw = {}; self.readers = {}

    def finish(self, eng='sp'):
        for b, ev in list(self.lastw.items()): self._wait(eng, ev)


def _mk_ident(nc, S, identf, identb):
    S.op('pool', lambda: nc.gpsimd.memset(identf[:], 0.0), writes=['identf'])
    S.op('pool', lambda: nc.gpsimd.affine_select(out=identf[:], in_=identf[:], pattern=[[-1, 128]],
                                                 compare_op=ALU.not_equal, fill=1.0, base=0,
                                                 channel_multiplier=1), reads=['identf'], writes=['identf'])
    S.op('dve', lambda: nc.vector.tensor_copy(out=identb[:], in_=identf[:]), reads=['identf'], writes=['identb'])


P_GROUPS = [('aq', 0, 512, 'rope', 'qTa', 1.0), ('ak', 512, 512, 'rope', 'kTa', 1.0), ('av', 1024, 512, 'v', 'va', 1.0),
            ('ag', 1536, 512, 'silu', 0, 1.0), ('bq', 2048, 512, 'rope', 'qTb', 1.0), ('bk', 2560, 512, 'rope', 'kTb', 1.0),
            ('bv', 3072, 512, 'v', 'vb', 1.0), ('bg', 3584, 512, 'silu', 1, 1.0), ('iq', 4096, 512, 'rope', 'qiT', 0.125),
            ('ik', 4608, 72, 'ik', None, 1.0), ('mq', 4680, 512, 'mq', 'mqT', 1.0), ('mg', 5192, 512, 'silu', 2, 1.0)] + \
           [('mix%d' % i, 5704 + 512 * i, 512, 'sig', i, 1.0) for i in range(6)]


def emit_P(nc, S, es, io, pfx=''):
    sb = lambda name, shape, dt: es.enter_context(nc.sbuf_tensor(pfx + "s_" + name, shape, dt))
    identf = sb("identf", [128, 128], F32); identb = sb("identb", [128, 128], BF16)
    _mk_ident(nc, S, identf, identb)
    gbc = sb("gbc", [128, D], F32); mgbc = sb("mgbc", [128, D], F32)
    S.dma('sp', gbc[:], io['g'].partition_broadcast(128), writes=['gbc'])
    S.dma('sp', mgbc[:], io['mg'].partition_broadcast(128), writes=['mgbc'])
    cos_t = sb("cos_t", [128, NSLOT, 8], F32); sin_t = sb("sin_t", [128, NSLOT, 8], F32)
    for j in range(NSLOT):
        S.dma('sp', cos_t[:, j, :], io['cos'][j * 128:(j + 1) * 128, :], writes=['cos_t'])
        S.dma('sp', sin_t[:, j, :], io['sin'][j * 128:(j + 1) * 128, :], writes=['sin_t'])
    hT = sb("hT", [128, NSLOT, 8, 128], BF16)
    hmT = sb("hmT", [128, 2, 8, 128], BF16)
    xt = [sb(f"xt{i}", [128, D], F32) for i in range(2)]
    junkf = sb("junkf", [128, D], F32)
    hb = [sb(f"hb{i}", [128, D], BF16) for i in range(2)]
    st = sb("st", [128, 8], F32)
    psT = [es.enter_context(nc.psum_tensor(pfx + f"psT{i}", [128, 8, 128], BF16)) for i in range(2)]
    psA = [es.enter_context(nc.psum_tensor(pfx + f"psA{i}", [128, 512], F32)) for i in range(4)]

    def norm_tile(src_ap, gb, gbk, dst_ap, dstk, i, srck=None):
        b = i % 2
        S.dma('sp', xt[b][:], src_ap, reads=([srck] if srck else []), writes=[f'xt{b}'])
        S.op('act', lambda: nc.scalar.activation(out=junkf[:], in_=xt[b][:], func=AF.Square, accum_out=st[:, 0:1]),
             reads=[f'xt{b}'], writes=['junkf', 'st0'])
        S.op('dve', lambda: nc.vector.tensor_scalar(out=st[:, 1:2], in0=st[:, 0:1], scalar1=1.0 / D, scalar2=1e-6,
                                                    op0=ALU.mult, op1=ALU.add), reads=['st0'], writes=['st1'])
        S.op('act', lambda: nc.scalar.activation(out=st[:, 2:3], in_=st[:, 1:2], func=AF.Sqrt), reads=['st1'], writes=['st2'])
        S.op('dve', lambda: nc.vector.reciprocal(out=st[:, 3:4], in_=st[:, 2:3]), reads=['st2'], writes=['st3'])
        S.op('dve', lambda: nc.vector.scalar_tensor_tensor(out=hb[b][:], in0=xt[b][:], scalar=st[:, 3:4], in1=gb[:],
                                                           op0=ALU.mult, op1=ALU.mult),
             reads=[f'xt{b}', 'st3', gbk], writes=[f'hb{b}'])
        for k in range(8):
            S.op('pe', lambda k=k: nc.tensor.transpose(psT[b][:, k, :], hb[b][:, k * 128:(k + 1) * 128], identb[:]),
                 reads=[f'hb{b}', 'identb'], writes=[f'psT{b}'])
        S.op('act', lambda: nc.scalar.activation(out=dst_ap, in_=psT[b][:], func=AF.Copy), reads=[f'psT{b}'], writes=[dstk])

    for j in range(NSLOT):
        norm_tile(io['x'][j * 128:(j + 1) * 128, :], gbc, 'gbc', hT[:, j, :, :], f'hT{j}', j, io['xkey'](j) if 'xkey' in io else None)
    for m in range(2):
        norm_tile(io['mem'][m * 128:(m + 1) * 128, :], mgbc, 'mgbc', hmT[:, m, :, :], f'hmT{m}', m)

    wbuf = [sb(f"wbuf{i}", [128, 8, 512], BF16) for i in range(2)]
    pr = [sb(f"pr{i}", [128, 512], F32) for i in range(2)]
    prb = [sb(f"prb{i}", [128, 512], BF16) for i in range(2)]
    rt = [sb(f"rt{i}", [128, 4, 8, 8], F32) for i in range(2)]
    oT = [sb(f"oT{i}", [128, 4, 128], BF16) for i in range(2)]
    oTk = [sb(f"oTk{i}", [64, 128], BF16) for i in range(2)]
    vt = [sb(f"vt{i}", [128, 8, 65], BF16) for i in range(2)]
    gt = [sb(f"gt{i}", [128, 512], BF16) for i in range(2)]
    wt = [sb(f"wt{i}", [128, 8], F32) for i in range(2)]
    for i in range(2):
        S.op('pool', lambda i=i: nc.gpsimd.memset(vt[i][:, :, 64:65], 1.0), writes=[f'vt{i}'])
    w_view = io['w_in'].rearrange("(k p) c -> p k c", p=128)
    cnt = {'ps': 0, 'it': 0}

    def rope(prt, prk, nh, j, rtb, rtk):
        v = prt.rearrange("p (h d) -> p h d", d=64)
        x1 = v[:, :, 0:8]; x2 = v[:, :, 8:16]
        cb = cos_t[:, j, :].unsqueeze(1).to_broadcast([128, nh, 8])
        sn = sin_t[:, j, :].unsqueeze(1).to_broadcast([128, nh, 8])
        for q, (a, bb) in enumerate([(x1, cb), (x2, sn), (x2, cb), (x1, sn)]):
            S.op('dve', lambda q=q, a=a, bb=bb: nc.vector.tensor_tensor(out=rtb[:, q, 0:nh, :], in0=a, in1=bb, op=ALU.mult),
                 reads=[prk, 'cos_t', 'sin_t'], writes=[rtk])
        S.op('dve', lambda: nc.vector.tensor_tensor(out=x1, in0=rtb[:, 0, 0:nh, :], in1=rtb[:, 1, 0:nh, :], op=ALU.subtract),
             reads=[rtk], writes=[prk])
        S.op('dve', lambda: nc.vector.tensor_tensor(out=x2, in0=rtb[:, 2, 0:nh, :], in1=rtb[:, 3, 0:nh, :], op=ALU.add),
             reads=[rtk], writes=[prk])

    def do_group(gidx, name, c0, n, kind, dst, scale, lhs, nslots, wv, is_mem=False, wtile=None, wkey=None):
        b = gidx % 2
        if wtile is None:
            S.dma('pool', wbuf[b][:, :, 0:n], wv[:, :, c0:c0 + n], writes=[f'wbuf{b}'])
            wtile = wbuf[b]; wkey = f'wbuf{b}'
        for j in range(nslots):
            pi = cnt['ps'] % 4; cnt['ps'] += 1
            it = cnt['it'] % 2; cnt['it'] += 1
            ps = psA[pi]; psk = f'psA{pi}'
            for k in range(8):
                S.op('pe', lambda k=k: nc.tensor.matmul(ps[:, 0:n], lhsT=lhs(j, k), rhs=wtile[:, k, 0:n],
                                                        start=(k == 0), stop=(k == 7)),
                     reads=[wkey, f'hT{j}' if not is_mem else f'hmT{j}'], writes=[psk])
            if kind in ('rope', 'mq', 'kmT'):
                if kind == 'rope':
                    S.op('act', lambda: nc.scalar.activation(out=pr[it][:], in_=ps[:], func=AF.Copy, scale=scale),
                         reads=[psk], writes=[f'pr{it}'])
                    rope(pr[it][:], f'pr{it}', 8, j, rt[it], f'rt{it}')
                    S.op('dve', lambda: nc.vector.tensor_copy(out=prb[it][:], in_=pr[it][:]), reads=[f'pr{it}'], writes=[f'prb{it}'])
                else:
                    S.op('act', lambda: nc.scalar.activation(out=prb[it][:], in_=ps[:], func=AF.Copy), reads=[psk], writes=[f'prb{it}'])
                tb = it
                for q in range(4):
                    S.op('pe', lambda q=q: nc.tensor.transpose(psT[tb][:, q, :], prb[it][:, q * 128:(q + 1) * 128], identb[:]),
                         reads=[f'prb{it}', 'identb'], writes=[f'psT{tb}'])
                if kind == 'kmT':
                    S.op('act', lambda: nc.scalar.activation(out=io['kmT_sb'][:, :, j * 128:(j + 1) * 128], in_=psT[tb][:, 0:4, :], func=AF.Copy),
                         reads=[f'psT{tb}'], writes=['kmT_sb'])
                else:
                    S.op('act', lambda: nc.scalar.activation(out=oT[it][:], in_=psT[tb][:, 0:4, :], func=AF.Copy),
                         reads=[f'psT{tb}'], writes=[f'oT{it}'])
                    S.dma('sp', io[dst][j * 128:(j + 1) * 128, :], oT[it][:].rearrange("p a b -> p (a b)"), reads=[f'oT{it}'], writes=[f'{dst}{j}'])
            elif kind == 'ik':
                S.op('act', lambda: nc.scalar.activation(out=pr[it][:, 0:72], in_=ps[:, 0:72], func=AF.Copy), reads=[psk], writes=[f'pr{it}'])
                rope(pr[it][:, 0:64], f'pr{it}', 1, j, rt[it], f'rt{it}')
                S.op('dve', lambda: nc.vector.tensor_copy(out=prb[it][:, 0:64], in_=pr[it][:, 0:64]), reads=[f'pr{it}'], writes=[f'prb{it}'])
                S.op('dve', lambda: nc.vector.tensor_scalar(out=wt[it][:], in0=pr[it][:, 64:72], scalar1=float(8 ** -0.5), scalar2=None, op0=ALU.mult),
                     reads=[f'pr{it}'], writes=[f'wt{it}'])
                S.dma('sp', io['wts'][j * 128:(j + 1) * 128, :], wt[it][:], reads=[f'wt{it}'], writes=[f'wts{j}'])
                S.op('pe', lambda: nc.tensor.transpose(psT[it][0:64, 0, :], prb[it][:, 0:64], identb[:]),
                     reads=[f'prb{it}', 'identb'], writes=[f'psT{it}'])
                S.op('act', lambda: nc.scalar.activation(out=oTk[it][:], in_=psT[it][0:64, 0, :], func=AF.Copy), reads=[f'psT{it}'], writes=[f'oTk{it}'])
                S.dma('sp', io['kiT'][j * 64:(j + 1) * 64, :], oTk[it][:], reads=[f'oTk{it}'], writes=[f'kiT{j}'])
            elif kind == 'v':
                S.op('act', lambda: nc.scalar.activation(out=vt[it][:, :, 0:64], in_=ps[:].rearrange("p (h d) -> p h d", d=64), func=AF.Copy),
                     reads=[psk], writes=[f'vt{it}'])
                S.dma('sp', io[dst][j * 128:(j + 1) * 128, :], vt[it][:].rearrange("p a b -> p (a b)"), reads=[f'vt{it}'], writes=[f'{dst}{j}'])
            elif kind == 'vm':
                S.op('act', lambda: nc.scalar.activation(out=io['vma_sb'][:, j, :, 0:128], in_=ps[:].rearrange("p (h d) -> p h d", d=128), func=AF.Copy),
                     reads=[psk], writes=['vma_sb'])
            elif kind in ('silu', 'sig'):
                fn = AF.Silu if kind == 'silu' else AF.Sigmoid
                S.op('act', lambda: nc.scalar.activation(out=gt[it][:], in_=ps[:], func=fn), reads=[psk], writes=[f'gt{it}'])
                dd = io['gates'] if kind == 'silu' else io['mix']
                S.dma('sp', dd[j * 128:(j + 1) * 128, dst * 512:(dst + 1) * 512], gt[it][:], reads=[f'gt{it}'],
                      writes=[('gates%d_' % dst if kind == 'silu' else 'mix%d_' % dst) + str(j)])

    gi = 0
    order = [1, 5, 9, 2, 6] + [i for i in range(len(P_GROUPS)) if i not in (1, 5, 9, 2, 6)]
    wrem = [sb(f"wrem{i}", [128, 8, 512], BF16) for i in range(len(order) - 5)]
    for oi, gidx_ in enumerate(order):
        (name, c0, n, kind, dst, scale) = P_GROUPS[gidx_]
        if oi < 5:
            do_group(gi, name, c0, n, kind, dst, scale, lambda j, k: hT[:, j, k, :], NSLOT, w_view)
        else:
            do_group(gi, name, c0, n, kind, dst, scale, lambda j, k: hT[:, j, k, :], NSLOT, w_view, wtile=wrem[oi - 5], wkey=f'wrem{oi - 5}')
        gi += 1
        if oi == 4:
            for oj in range(5, len(order)):
                (_, c0_, n_, _, _, _) = P_GROUPS[order[oj]]
                S.dma('pool', wrem[oj - 5][:, :, 0:n_], w_view[:, :, c0_:c0_ + n_], writes=[f'wrem{oj - 5}'])
            if 'after_kv' in io: io['after_kv']()
    io['kmT_sb'] = sb("kmT_sb", [128, 4, 256], BF16)
    io['vma_sb'] = sb("vma_sb", [128, 2, 4, 129], BF16)
    S.op('pool', lambda: nc.gpsimd.memset(io['vma_sb'][:, :, :, 128:129], 1.0), writes=['vma_sb'])
    wkv_view = io['wkv'].rearrange("(k p) c -> p k c", p=128)
    do_group(gi, 'km', 0, 512, 'kmT', None, 1.0, lambda j, k: hmT[:, j, k, :], 2, wkv_view, is_mem=True); gi += 1
    do_group(gi, 'vm', 512, 512, 'vm', None, 1.0, lambda j, k: hmT[:, j, k, :], 2, wkv_view, is_mem=True); gi += 1
    S.dma('sp', io['kmT'], io['kmT_sb'][:].rearrange("p a b -> p (a b)"), reads=['kmT_sb'], writes=['kmT'])
    S.dma('sp', io['vma'], io['vma_sb'][:].rearrange("p a b c -> p (a b c)"), reads=['vma_sb'], writes=['vma'])


P_IN = [('x', [TOK, D], F32), ('cos', [TOK, 8], F32), ('sin', [TOK, 8], F32), ('g', [1, D], F32), ('mg', [1, D], F32),
        ('w_in', [D, INC], F32), ('mem', [256, D], F32), ('wkv', [D, D], F32)]
P_OUT = [('qTa', [TOK, 512], BF16), ('qTb', [TOK, 512], BF16), ('qiT', [TOK, 512], BF16), ('mqT', [TOK, 512], BF16),
         ('kTa', [TOK, 512], BF16), ('kTb', [TOK, 512], BF16), ('kiT', [NSLOT * 64, 128], BF16),
         ('va', [TOK, 520], BF16), ('vb', [TOK, 520], BF16), ('gates', [TOK, 1536], BF16), ('wts', [TOK, 8], F32),
         ('mix', [TOK, 3072], BF16), ('kmT', [128, 1024], BF16), ('vma', [128, 1032], BF16)]


def build_P():
    nc = bass.Bass("TRN2", target_bir_lowering=False)
    io = {}
    for n, s, d in P_IN: io[n] = nc.dram_tensor(n, s, d, kind="ExternalInput").ap()
    for n, s, d in P_OUT: io[n] = nc.dram_tensor(n, s, d, kind="ExternalOutput").ap()
    with ExitStack() as es:
        S = Sched(nc, es)
        emit_P(nc, S, es, io)
        S.finish('sp')
    return nc


def emit_A(nc, S, es, io, final, nrun=NSLOT, pfx=''):
    sb = lambda name, shape, dt: es.enter_context(nc.sbuf_tensor(pfx + "s_" + name, shape, dt))
    identf = sb("identf", [128, 128], F32); identb = sb("identb", [128, 128], BF16)
    _mk_ident(nc, S, identf, identb)
    wb = sb("wb", [128, 12, D], BF16); wo = sb("wo", [128, 8, D], BF16)
    wbv = io['w_branch'].rearrange("(c p) d -> p c d", p=128)
    wov = io['w_out'].rearrange("(c p) d -> p c d", p=128)
    for c in range(0, 12, 4):
        S.dma('pool', wb[:, c:c + 4, :], wbv[:, c:c + 4, :], writes=['wb'])
    for c in range(0, 8, 4):
        S.dma('pool', wo[:, c:c + 4, :], wov[:, c:c + 4, :], writes=['wo'])
    kmT = sb("kmT_a", [128, 4, 256], BF16); vma = sb("vma_a", [128, 2, 4, 129], BF16)
    S.dma('sp', kmT[:].rearrange("p a b -> p (a b)"), io['kmT'], reads=['kmT'], writes=['kmT_s'])
    S.dma('sp', vma[:].rearrange("p a b c -> p (a b c)"), io['vma'], reads=['vma'], writes=['vma_s'])
    cmf = sb("cmf", [128, 512], F32); cmb = sb("cmb", [128, 512], BF16)
    S.dma('sp', cmf[:], io['cm'], writes=['cmf'])
    S.op('dve', lambda: nc.vector.tensor_copy(out=cmb[:], in_=cmf[:]), reads=['cmf'], writes=['cmb'])
    pastm = sb("pastm", [128, NSLOT, 32], F32); ispm = sb("ispm", [128, NSLOT, 32], F32); ownfix = sb("ownfix", [128, NSLOT, 32], F32)
    for t, k in ((pastm, 'pastm'), (ispm, 'ispm'), (ownfix, 'ownfix')):
        S.dma('sp', t[:].rearrange("p a b -> p (a b)"), io[k], writes=[k])
    fgbc = sb("fgbc", [128, D], F32)
    S.dma('sp', fgbc[:], io['fg'].partition_broadcast(128), writes=['fgbc'])
    halves = sb("halves", [128, NBIS], F32)
    for k in range(NBIS):
        S.op('pool', lambda k=k: nc.gpsimd.memset(halves[:, k:k + 1], float(2.0 ** -(k + 1))), writes=['halves'])

    kg = [sb(f"kg{i}", [128, 4, 512], BF16) for i in range(2)]
    vg = [sb(f"vg{i}", [128, 4, 520], BF16) for i in range(2)]
    kig = [sb(f"kig{i}", [128, 4, 128], BF16) for i in range(2)]
    R = [sb(f"R{i}", [128, 512], BF16) for i in range(3)]
    PT = [sb(f"PT{i}", [128, 512], BF16) for i in range(3)]
    I_all = sb("I_all", [128, SEQ], F32)
    selb = sb("selb", [128, SEQ], BF16)
    junk16 = sb("junk16", [128, SEQ], BF16)
    psS = [es.enter_context(nc.psum_tensor(pfx + f"psS{i}", [128, 512], F32)) for i in range(2)]
    psI = [es.enter_context(nc.psum_tensor(pfx + f"psI{i}", [128, 512], F32)) for i in range(1)]
    psOa = [es.enter_context(nc.psum_tensor(pfx + f"psOa{i}", [128, 512], F32)) for i in range(2)]
    psOb = [es.enter_context(nc.psum_tensor(pfx + f"psOb{i}", [128, 512], F32)) for i in range(2)]
    psMb = es.enter_context(nc.psum_tensor(pfx + "psMb", [128, 1024], BF16))
    psM = psI[0]

    class _GV:
        def __init__(self, name, p):
            self.name = name; self.p = p
        def __getitem__(self, gi):
            if self.name + '_g' in io:
                return io[self.name + '_g'].rearrange("(r j p) c -> j p r c", r=4, j=NSLOT, p=self.p)[gi]
            for ci, (s0, ns) in enumerate(GCHUNKS[self.name]):
                if s0 <= gi < s0 + ns:
                    return io[f'{self.name}_g{ci}'].rearrange("(r j p) c -> j p r c", r=4, j=ns, p=self.p)[gi - s0]
    kTa_v = _GV('kTa', 128); kTb_v = _GV('kTb', 128); va_v = _GV('va', 128); vb_v = _GV('vb', 128); ki_v = _GV('kiT', 64)

    ksum = sb("ksum", [128, 64, 4], F32)
    km32 = sb("km32", [128, 32, 4], F32)
    kmeanT = sb("kmeanT", [128, 4, 32], BF16)
    for gi in range(NSLOT):
        b = gi % 2
        S.dma('sp', kg[b][:], kTa_v[gi], reads=['kTa_g'], writes=[f'kg{b}'])
        S.op('dve', lambda gi=gi, b=b: nc.vector.tensor_reduce(
            out=ksum[:, gi * 4:(gi + 1) * 4, :], in_=kg[b][:].rearrange("p c (a t) -> p c a t", t=128), axis=AX.X, op=ALU.add),
            reads=[f'kg{b}'], writes=['ksum'])
    ksv = ksum[:].rearrange("p (b two) a -> p b two a", two=2)
    S.op('dve', lambda: nc.vector.tensor_tensor(out=km32[:], in0=ksv[:, :, 0, :], in1=ksv[:, :, 1, :], op=ALU.add), reads=['ksum'], writes=['km32'])
    S.op('dve', lambda: nc.vector.tensor_scalar(out=kmeanT[:].rearrange("p a b -> p b a"), in0=km32[:], scalar1=1.0 / 256, scalar2=None, op0=ALU.mult),
         reads=['km32'], writes=['kmeanT'])

    _ck('init')
    qTa = sb("qTa_t", [128, 8, 128], BF16); qTb = sb("qTb_t", [128, 8, 128], BF16)
    qiT = sb("qiT_t", [128, 8, 128], BF16); mqT = sb("mqT_t", [128, 4, 128], BF16)
    for t_, k_ in ((qTa, 'qTa'), (qTb, 'qTb'), (qiT, 'qiT')):
        S.op('pool', lambda t_=t_: nc.gpsimd.memset(t_[:], 0.0), writes=[k_ + '_ta', k_ + '_tb'])
    wts = sb("wts_t", [128, 8], F32); gA = sb("gA_t", [128, 512], BF16); gBM = sb("gBM_t", [128, 1024], BF16); mix = sb("mix_t", [128, 3072], BF16)
    xt = sb("xt_t", [128, D], F32)
    diagw = sb("diagw", [128, 8, 128], BF16)
    bs = sb("bs", [128, 16], F32)
    steps = sb("steps", [128, NBIS], F32)
    gm = sb("gm", [128, 8, 32], F32); top8 = sb("top8", [128, 8, 8], F32); gsel = sb("gsel", [128, 8, 32], F32)
    gbb = sb("gbb", [128, 8, 32], BF16)
    rec = sb("rec", [128, 8], F32); yf = sb("yf", [128, 512], F32)
    ycat = sb("ycat", [128, 3, 512], BF16)
    yT = sb("yT", [128, 12, 128], BF16)
    merged = sb("merged", [128, D], F32); mtmp = sb("mtmp", [128, 512], F32); mergb = sb("mergb", [128, D], BF16)
    mT = sb("mT", [128, 8, 128], BF16)
    xo = sb("xo", [128, D], F32)
    rot = {'s': 0, 'p': 0, 'r': 0}

    def hp(h):
        return slice((h % 2) * 64, (h % 2) * 64 + 64)

    def mk(j):
        G = j + 1
        n = 512 * G
        rows = slice(j * 128, (j + 1) * 128)

        def loads_AB():
            for t, k in ((qiT, 'qiT'), (qTa, 'qTa')):
                tv = t[:].rearrange("p (a two) t -> p a two t", two=2)
                S.dma('sp', tv[0:64, :, 0, :], io[k][j * 128:j * 128 + 64, :].rearrange("p (a t) -> p a t", t=128), reads=[f'{k}{j}'], writes=[k + '_ta'])
                S.dma('sp', tv[64:128, :, 1, :], io[k][j * 128 + 64:j * 128 + 128, :].rearrange("p (a t) -> p a t", t=128), reads=[f'{k}{j}'], writes=[k + '_tb'])
                if k == 'qiT':
                    S.dma('sp', wts[:], io['wts'][rows, :], reads=[f'wts{j}'], writes=['wts_t'])

        def load_gA():
            S.dma('sp', gA[:], io['gates'][rows, 0:512], reads=[f'gates0_{j}'], writes=['gA_t'])

        def loads_C():
            tv = qTb[:].rearrange("p (a two) t -> p a two t", two=2)
            S.dma('sp', tv[0:64, :, 0, :], io['qTb'][j * 128:j * 128 + 64, :].rearrange("p (a t) -> p a t", t=128), reads=[f'qTb{j}'], writes=['qTb_ta'])
            S.dma('sp', tv[64:128, :, 1, :], io['qTb'][j * 128 + 64:j * 128 + 128, :].rearrange("p (a t) -> p a t", t=128), reads=[f'qTb{j}'], writes=['qTb_tb'])
            S.dma('sp', mqT[:].rearrange("p a b -> p (a b)"), io['mqT'][rows, :], reads=[f'mqT{j}'], writes=['mqT_t'])
            S.dma('sp', gBM[:], io['gates'][rows, 512:1536], reads=[f'gates1_{j}', f'gates2_{j}'], writes=['gBM_t'])
            S.dma('sp', mix[:], io['mix'][rows, :], reads=[f'mix{n_}_{j}' for n_ in range(6)], writes=['mix_t'])
            S.dma('sp', xt[:], io['x'][rows, :], reads=([io['xkey'](j)] if 'xkey' in io else []), writes=['xt_t'])

        def diag():
            for h in range(8):
                S.op('dve', lambda h=h: nc.vector.tensor_scalar(out=diagw[:, h, :], in0=identf[:], scalar1=wts[:, h:h + 1], scalar2=None, op0=ALU.mult),
                     reads=['identf', 'wts_t'], writes=['diagw'])


        def idx():
            def ld_ki(gi):
                b = gi % 2
                S.dma('sp', kig[b][0:64, :, :], ki_v[gi], reads=['kiT_g'], writes=[f'kig{b}a'])
                S.dma('sp', kig[b][64:128, :, :], ki_v[gi], reads=['kiT_g'], writes=[f'kig{b}b'])
            items = [(gi, h) for gi in range(G) for h in range(8)]
            sbank = {}

            def emit_S(i):
                gi, h = items[i]
                si = rot['s'] % 2; rot['s'] += 1; sbank[i] = si
                S.op('pe', lambda: nc.tensor.matmul(psS[si][:], lhsT=qiT[:, h, :],
                                                    rhs=kig[gi % 2][:].rearrange("p a b -> p (a b)"), start=True, stop=True),
                     reads=['qiT_ta', 'qiT_tb', f'kig{gi % 2}a', f'kig{gi % 2}b'], writes=[f'psS{si}'])
            ld_ki(0)
            emit_S(0)
            for i, (gi, h) in enumerate(items):
                if h == 0 and gi + 1 < G: ld_ki(gi + 1)
                if i + 1 < len(items): emit_S(i + 1)
                si = sbank[i]; ri = rot['r'] % 3; rot['r'] += 1
                S.op('act', lambda: nc.scalar.activation(out=R[ri][:], in_=psS[si][:], func=AF.Relu), reads=[f'psS{si}'], writes=[f'R{ri}'])
                ib = 0
                S.op('pe', lambda: nc.tensor.matmul(psI[ib][:], lhsT=diagw[:, h, :], rhs=R[ri][:], start=(h == 0), stop=(h == 7)),
                     reads=['diagw', f'R{ri}'], writes=[f'psI{ib}'])
                if h == 7:
                    dst = I_all[:, gi * 512:(gi + 1) * 512]
                    if gi == j:
                        S.op('dve', lambda: nc.vector.tensor_tensor(out=dst, in0=psI[ib][:], in1=cmf[:], op=ALU.add),
                             reads=[f'psI{ib}', 'cmf'], writes=['I_all'])
                    else:
                        S.op('dve', lambda: nc.vector.tensor_copy(out=dst, in_=psI[ib][:]), reads=[f'psI{ib}'], writes=['I_all'])


        def gate():
            for h in range(8):
                S.op('pe', lambda h=h: nc.tensor.matmul(psI[0][:, h * 32:(h + 1) * 32], lhsT=qTa[:, h, :], rhs=kmeanT[:, h // 2, :],
                                                        start=(h == 0), stop=(h == 7), skip_group_check=True),
                     reads=['qTa_ta', 'qTa_tb', 'kmeanT'], writes=['psI0'])
            S.op('dve', lambda: nc.vector.tensor_tensor(out=gm[:], in0=psI[0][:, 0:256].rearrange("p (h b) -> p h b", b=32),
                                                        in1=pastm[:, j, :].unsqueeze(1).to_broadcast([128, 8, 32]), op=ALU.add),
                 reads=['psI0', 'pastm'], writes=['gm'])
            for h in range(8):
                S.op('dve', lambda h=h: nc.vector.max(out=top8[:, h, :], in_=gm[:, h, :]), reads=['gm'], writes=['top8'])
            S.op('dve', lambda: nc.vector.tensor_tensor(out=gsel[:], in0=gm[:], in1=top8[:, :, 2:3].to_broadcast([128, 8, 32]), op=ALU.is_lt),
                 reads=['gm', 'top8'], writes=['gsel'])
            S.op('dve', lambda: nc.vector.tensor_tensor(out=gsel[:], in0=gsel[:], in1=ispm[:, j, :].unsqueeze(1).to_broadcast([128, 8, 32]), op=ALU.mult),
                 reads=['gsel', 'ispm'], writes=['gsel'])
            S.op('dve', lambda: nc.vector.tensor_tensor(out=gbb[:], in0=gsel[:], in1=ownfix[:, j, :].unsqueeze(1).to_broadcast([128, 8, 32]), op=ALU.add),
                 reads=['gsel', 'ownfix'], writes=['gbb'])


        def bisect():
            Iv = I_all[:, 0:n]
            S.op('dve', lambda: nc.vector.tensor_reduce(out=bs[:, 0:1], in_=Iv, axis=AX.X, op=ALU.max), reads=['I_all'], writes=['bs0'])
            S.op('dve', lambda: nc.vector.scalar_tensor_tensor(out=mtmp[:], in0=cmf[:], scalar=-2.0, in1=I_all[:, n - 512:n], op0=ALU.mult, op1=ALU.add),
                 reads=['cmf', 'I_all'], writes=['mtmp'])
            S.op('dve', lambda: nc.vector.tensor_reduce(out=bs[:, 1:2], in_=mtmp[:], axis=AX.X, op=ALU.min), reads=['mtmp'], writes=['bs1'])
            if G > 1:
                S.op('dve', lambda: nc.vector.tensor_reduce(out=bs[:, 2:3], in_=I_all[:, 0:n - 512], axis=AX.X, op=ALU.min), reads=['I_all'], writes=['bs2'])
                S.op('dve', lambda: nc.vector.tensor_tensor(out=bs[:, 1:2], in0=bs[:, 1:2], in1=bs[:, 2:3], op=ALU.min), reads=['bs1', 'bs2'], writes=['bs1'])
            S.op('dve', lambda: nc.vector.scalar_tensor_tensor(out=bs[:, 3:4], in0=bs[:, 0:1], scalar=1.0, in1=bs[:, 1:2], op0=ALU.add, op1=ALU.subtract),
                 reads=['bs0', 'bs1'], writes=['bs3'])
            S.op('dve', lambda: nc.vector.tensor_scalar(out=steps[:], in0=halves[:], scalar1=bs[:, 3:4], scalar2=None, op0=ALU.mult),
                 reads=['halves', 'bs3'], writes=['steps'])
            for k in range(NBIS):
                S.op('dve', lambda k=k: nc.vector.tensor_tensor(out=bs[:, 4:5], in0=bs[:, 1:2], in1=steps[:, k:k + 1], op=ALU.add),
                     reads=['bs1', 'steps'], writes=['bs4'])
                S.op('dve', lambda: nc.vector.tensor_scalar(out=junk16[:, 0:n], in0=Iv, scalar1=bs[:, 4:5], scalar2=None, op0=ALU.is_ge, op1=ALU.add,
                                                            accum_out=bs[:, 5:6]), reads=['I_all', 'bs4'], writes=['junk16', 'bs5'])
                S.op('dve', lambda k=k: nc.vector.tensor_scalar(out=bs[:, 6:7], in0=bs[:, 5:6], scalar1=255.5, scalar2=steps[:, k:k + 1], op0=ALU.is_ge, op1=ALU.mult),
                     reads=['bs5', 'steps'], writes=['bs6'])
                S.op('dve', lambda: nc.vector.tensor_tensor(out=bs[:, 1:2], in0=bs[:, 1:2], in1=bs[:, 6:7], op=ALU.add), reads=['bs1', 'bs6'], writes=['bs1'])

        def bisect_final():
            Iv = I_all[:, 0:n]
            S.op('dve', lambda: nc.vector.tensor_scalar(out=selb[:, 0:n], in0=Iv, scalar1=bs[:, 1:2], scalar2=NEG, op0=ALU.is_lt, op1=ALU.mult),
                 reads=['I_all', 'bs1'], writes=['selb'])


        def attn_pass(kview, vview, qT, qk, kind, kgk, vgk, pso, psok):
            itm = [(gi, h) for gi in range(G) for h in range(8)]
            sbk = {}

            def ld(gi):
                b = gi % 2
                S.dma('sp', kg[b][:], kview[gi], reads=[kgk], writes=[f'kg{b}'])
                S.dma('sp', vg[b][:], vview[gi], reads=[vgk], writes=[f'vg{b}'])

            def emit_qk(i):
                gi, h = itm[i]
                si = rot['s'] % 2; rot['s'] += 1; sbk[i] = si
                b = gi % 2
                first = True
                for cc in range(4):
                    mms = []
                    mms.append((kg[b][:, cc, (h // 2) * 128:(h // 2) * 128 + 128], qT[:, h, :], [f'kg{b}', qk + 'a', qk + 'b']))
                    if kind == 'moba':
                        blk = 2 * gi + cc // 2
                        mms.append((gbb[:, h, blk:blk + 1].to_broadcast([128, 128]), identb[:], ['gbb', 'identb']))
                        if gi == j:
                            mms.append((cmb[:, cc * 128:(cc + 1) * 128], identb[:], ['cmb', 'identb']))
                    else:
                        c = 4 * gi + cc
                        mms.append((selb[:, c * 128:(c + 1) * 128], identb[:], ['selb', 'identb']))
                    for mi, (l, r, rd) in enumerate(mms):
                        last = (cc == 3 and mi == len(mms) - 1)
                        S.op('pe', lambda l=l, r=r, first=first, last=last: nc.tensor.matmul(
                            psS[si][:, cc * 128:(cc + 1) * 128], lhsT=l, rhs=r, start=first, stop=last, skip_group_check=True),
                            reads=rd, writes=[f'psS{si}'])
                        first = False
            ld(0)
            emit_qk(0)
            for i, (gi, h) in enumerate(itm):
                if h == 0 and gi + 1 < G: ld(gi + 1)
                if i + 1 < len(itm): emit_qk(i + 1)
                si = sbk[i]; pi = rot['p'] % 3; rot['p'] += 1
                S.op('act', lambda: nc.scalar.activation(out=PT[pi][:], in_=psS[si][:], func=AF.Exp, scale=0.125),
                     reads=[f'psS{si}'], writes=[f'PT{pi}'])
                ob = h // 4
                for cc in range(4):
                    first = (gi == 0 and cc == 0 and h % 4 == 0)
                    last = (gi == G - 1 and cc == 3 and h % 4 == 3)
                    S.op('pe', lambda cc=cc, first=first, last=last: nc.tensor.matmul(
                        pso[ob][:, (h % 4) * 65:(h % 4) * 65 + 65], lhsT=PT[pi][:, cc * 128:(cc + 1) * 128],
                        rhs=vg[gi % 2][:, cc, h * 65:(h + 1) * 65], start=first, stop=last, skip_group_check=True),
                        reads=[f'PT{pi}', f'vg{gi % 2}'], writes=[f'{psok}{ob}'])

        def finish_pass(nb, nh_per, dh, banks, bkeys, yout, youtk, gap, gak):
            for bi, (bank, bk) in enumerate(zip(banks, bkeys)):
                v = bank[:, 0:nh_per * (dh + 1)].rearrange("p (h e) -> p h e", e=dh + 1)
                S.op('dve', lambda v=v, bi=bi: nc.vector.reciprocal(out=rec[:, bi * nh_per:(bi + 1) * nh_per], in_=v[:, :, dh]),
                     reads=[bk], writes=['rec'])
                S.op('dve', lambda v=v, bi=bi: nc.vector.tensor_tensor(
                    out=yf[:, bi * nh_per * dh:(bi + 1) * nh_per * dh].rearrange("p (h d) -> p h d", d=dh), in0=v[:, :, 0:dh],
                    in1=rec[:, bi * nh_per:(bi + 1) * nh_per].unsqueeze(2).to_broadcast([128, nh_per, dh]), op=ALU.mult),
                    reads=[bk, 'rec'], writes=['yf'])
            S.op('dve', lambda: nc.vector.tensor_tensor(out=yout, in0=yf[:], in1=gap, op=ALU.mult),
                 reads=['yf', gak], writes=[youtk])


        def moba():
            attn_pass(kTa_v, va_v, qTa, 'qTa_t', 'moba', 'kTa_g', 'va_g', psOa, 'psOa')

        def fin_moba():
            finish_pass(0, 4, 64, psOa, ['psOa0', 'psOa1'], ycat[:, 0, :], 'ycat0', gA[:], 'gA_t')

        def dsa():
            attn_pass(kTb_v, vb_v, qTb, 'qTb_t', 'dsa', 'kTb_g', 'vb_g', psOb, 'psOb')

        def fin_dsa():
            finish_pass(1, 4, 64, psOb, ['psOb0', 'psOb1'], ycat[:, 1, :], 'ycat1', gBM[:, 0:512], 'gBM_t')

        def mem():
            for h in range(4):
                si = rot['s'] % 2; rot['s'] += 1
                for mc in range(2):
                    S.op('pe', lambda mc=mc: nc.tensor.matmul(psS[si][:, mc * 128:(mc + 1) * 128], lhsT=kmT[:, h, mc * 128:(mc + 1) * 128],
                                                              rhs=mqT[:, h, :], start=(mc == 0), stop=(mc == 1), skip_group_check=True),
                         reads=['kmT_s', 'mqT_t'], writes=[f'psS{si}'])
                pi = rot['p'] % 3; rot['p'] += 1
                S.op('act', lambda: nc.scalar.activation(out=PT[pi][:, 0:256], in_=psS[si][:, 0:256], func=AF.Exp, scale=float(128 ** -0.5)),
                     reads=[f'psS{si}'], writes=[f'PT{pi}'])
                ob = h // 2
                for mc in range(2):
                    S.op('pe', lambda mc=mc: nc.tensor.matmul(psOb[ob][:, (h % 2) * 129:(h % 2) * 129 + 129], lhsT=PT[pi][:, mc * 128:(mc + 1) * 128],
                                                              rhs=vma[:, mc, h, :], start=(mc == 0 and h % 2 == 0), stop=(mc == 1 and h % 2 == 1),
                                                              skip_group_check=True),
                         reads=[f'PT{pi}', 'vma_s'], writes=[f'psOb{ob}'])
            finish_pass(2, 2, 128, psOb, ['psOb0', 'psOb1'], ycat[:, 2, :], 'ycat2', gBM[:, 512:1024], 'gBM_t')


        def combine():
            ycf = ycat[:].rearrange("p a b -> p (a b)")
            for c0, c1 in ((0, 8), (8, 12)):
                for c in range(c0, c1):
                    S.op('pe', lambda c=c: nc.tensor.transpose(psMb[:, (c % 8) * 128:(c % 8) * 128 + 128], ycf[:, c * 128:(c + 1) * 128], identb[:]),
                         reads=['ycat0', 'ycat1', 'ycat2', 'identb'], writes=['psM'])
                S.op('act', lambda: nc.scalar.activation(out=yT[:, c0:c1, :].rearrange("p a b -> p (a b)"), in_=psMb[:, 0:(c1 - c0) * 128], func=AF.Copy),
                     reads=['psM'], writes=['yT'])
            for nb in range(3):
                for half in range(2):
                    si = rot['s'] % 2; rot['s'] += 1
                    for c in range(4):
                        S.op('pe', lambda c=c: nc.tensor.matmul(psS[si][:], lhsT=yT[:, nb * 4 + c, :], rhs=wb[:, nb * 4 + c, half * 512:(half + 1) * 512],
                                                                start=(c == 0), stop=(c == 3)),
                             reads=['yT', 'wb'], writes=[f'psS{si}'])
                    mg_ = mix[:, nb * 1024 + half * 512: nb * 1024 + (half + 1) * 512]
                    md = merged[:, half * 512:(half + 1) * 512]
                    if nb == 0:
                        S.op('dve', lambda: nc.vector.tensor_tensor(out=md, in0=psS[si][:], in1=mg_, op=ALU.mult),
                             reads=[f'psS{si}', 'mix_t'], writes=[f'merged{half}'])
                    else:
                        S.op('dve', lambda: nc.vector.tensor_tensor(out=mtmp[:], in0=psS[si][:], in1=mg_, op=ALU.mult),
                             reads=[f'psS{si}', 'mix_t'], writes=['mtmp'])
                        S.op('dve', lambda: nc.vector.tensor_tensor(out=md, in0=md, in1=mtmp[:], op=ALU.add),
                             reads=['mtmp', f'merged{half}'], writes=[f'merged{half}'])
            S.op('dve', lambda: nc.vector.tensor_copy(out=mergb[:], in_=merged[:]), reads=['merged0', 'merged1'], writes=['mergb'])
            for c in range(8):
                S.op('pe', lambda c=c: nc.tensor.transpose(psMb[:, c * 128:(c + 1) * 128], mergb[:, c * 128:(c + 1) * 128], identb[:]),
                     reads=['mergb', 'identb'], writes=['psM'])
            S.op('act', lambda: nc.scalar.activation(out=mT[:].rearrange("p a b -> p (a b)"), in_=psMb[:, 0:1024], func=AF.Copy),
                 reads=['psM'], writes=['mT'])
            for half in range(2):
                si = rot['s'] % 2; rot['s'] += 1
                for c in range(8):
                    S.op('pe', lambda c=c: nc.tensor.matmul(psS[si][:], lhsT=mT[:, c, :], rhs=wo[:, c, half * 512:(half + 1) * 512],
                                                            start=(c == 0), stop=(c == 7)),
                         reads=['mT', 'wo'], writes=[f'psS{si}'])
                S.op('dve', lambda: nc.vector.tensor_tensor(out=xo[:, half * 512:(half + 1) * 512], in0=psS[si][:], in1=xt[:, half * 512:(half + 1) * 512], op=ALU.add),
                     reads=[f'psS{si}', 'xt_t'], writes=[f'xo{half}'])
            if final:
                S.op('act', lambda: nc.scalar.activation(out=mergb[:], in_=xo[:], func=AF.Square, accum_out=bs[:, 8:9]),
                     reads=['xo0', 'xo1'], writes=['mergb', 'bs8'])
                S.op('dve', lambda: nc.vector.tensor_scalar(out=bs[:, 9:10], in0=bs[:, 8:9], scalar1=1.0 / D, scalar2=1e-6, op0=ALU.mult, op1=ALU.add),
                     reads=['bs8'], writes=['bs9'])
                S.op('act', lambda: nc.scalar.activation(out=bs[:, 10:11], in_=bs[:, 9:10], func=AF.Sqrt), reads=['bs9'], writes=['bs10'])
                S.op('dve', lambda: nc.vector.reciprocal(out=bs[:, 11:12], in_=bs[:, 10:11]), reads=['bs10'], writes=['bs11'])
                S.op('dve', lambda: nc.vector.scalar_tensor_tensor(out=xo[:], in0=xo[:], scalar=bs[:, 11:12], in1=fgbc[:], op0=ALU.mult, op1=ALU.mult),
                     reads=['xo0', 'xo1', 'bs11', 'fgbc'], writes=['xo0', 'xo1'])
                S.dma('pool', io['xout'][rows, :], xo[:], reads=['xo0', 'xo1'], writes=[io['xokey'](j) if 'xokey' in io else 'xout'])
            else:
                S.dma('pool', io['xout'][rows, :], xo[:], reads=['xo0', 'xo1'], writes=[io['xokey'](j) if 'xokey' in io else 'xout'])

        return dict(loads_AB=loads_AB, load_gA=load_gA, loads_C=loads_C, diag=diag, idx=idx, gate=gate, bisect=bisect, bisect_final=bisect_final, moba=moba, fin_moba=fin_moba,
                    dsa=dsa, fin_dsa=fin_dsa, mem=mem, combine=combine)

    parts = [mk(j) for j in range(nrun)]
    for j in range(nrun):
        p = parts[j]
        if j == 0:
            p['loads_AB'](); p['diag'](); p['idx']()
        p['gate'](); p['bisect']()
        if j > 0: parts[j - 1]['dsa']()
        p['bisect_final']()
        p['moba']()
        if j > 0: parts[j - 1]['fin_dsa']()
        if j + 1 < nrun:
            nx = parts[j + 1]; nx['loads_AB'](); nx['diag'](); nx['idx']()
        if j > 0:
            q = parts[j - 1]; q['mem'](); q['combine']()
        p['load_gA'](); p['fin_moba'](); p['loads_C']()
    q = parts[nrun - 1]; q['dsa'](); q['fin_dsa'](); q['mem'](); q['combine']()


A_IN = [('x', [TOK, D], F32), ('qTa', [TOK, 512], BF16), ('qTb', [TOK, 512], BF16), ('qiT', [TOK, 512], BF16), ('mqT', [TOK, 512], BF16),
        ('wts', [TOK, 8], F32), ('gates', [TOK, 1536], BF16), ('mix', [TOK, 3072], BF16),
        ('kTa_g', [4 * TOK, 512], BF16), ('kTb_g', [4 * TOK, 512], BF16), ('kiT_g', [4 * NSLOT * 64, 128], BF16),
        ('va_g', [4 * TOK, 520], BF16), ('vb_g', [4 * TOK, 520], BF16), ('kmT', [128, 1024], BF16), ('vma', [128, 1032], BF16),
        ('w_branch', [1536, D], F32), ('w_out', [D, D], F32), ('cm', [128, 512], F32),
        ('pastm', [128, NSLOT * 32], F32), ('ispm', [128, NSLOT * 32], F32), ('ownfix', [128, NSLOT * 32], F32), ('fg', [1, D], F32)]


def build_A(final, nrun=NSLOT):
    nc = bass.Bass("TRN2", target_bir_lowering=False)
    io = {}
    for n, s, d in A_IN: io[n] = nc.dram_tensor(n, s, d, kind="ExternalInput").ap()
    io['xout'] = nc.dram_tensor('xout', [TOK, D], F32, kind="ExternalOutput").ap()
    with ExitStack() as es:
        S = Sched(nc, es)
        try:
            emit_A(nc, S, es, io, final, nrun)
        except _StopEmit:
            pass
        print('A nins', S.nins, 'nwait', S.nwait, 'nsem', S.nsem)
        S.finish('sp')
    return nc


F_IN = [('x', [TOK, D], F32), ('cos', [TOK, 8], F32), ('sin', [TOK, 8], F32), ('g_all', [DEPTH, D], F32), ('mg_all', [DEPTH, D], F32),
        ('w_in_all', [DEPTH * D, INC], F32), ('mem', [256, D], F32), ('wkv_all', [DEPTH * D, D], F32),
        ('w_branch_all', [DEPTH * 1536, D], F32), ('w_out_all', [DEPTH * D, D], F32), ('cm', [128, 512], F32),
        ('pastm', [128, NSLOT * 32], F32), ('ispm', [128, NSLOT * 32], F32), ('ownfix', [128, NSLOT * 32], F32), ('fg', [1, D], F32)]
GATHER = [('kTa', 512, 128), ('kTb', 512, 128), ('kiT', 128, 64), ('va', 520, 128), ('vb', 520, 128)]
GCHUNKS = {'kTa': [(0, 8), (8, 8)], 'kTb': [(0, 8), (8, 8)], 'kiT': [(0, 16)], 'va': [(0, 7), (7, 7), (14, 2)], 'vb': [(0, 7), (7, 7), (14, 2)]}


def build_fused(depth=DEPTH, nrun=NSLOT):
    nc = bass.Bass("TRN2", target_bir_lowering=False)
    ext = {}
    for n, s_, d in F_IN: ext[n] = nc.dram_tensor(n, s_, d, kind="ExternalInput").ap()
    ext['xout'] = nc.dram_tensor('xout', [TOK, D], F32, kind="ExternalOutput").ap()
    scr = {}
    for n, s_, d in P_OUT: scr[n] = nc.dram_tensor("scr_" + n, s_, d).ap()
    for n, w, p in GATHER:
        for ci, (s0, ns) in enumerate(GCHUNKS[n]):
            scr[f'{n}_g{ci}'] = nc.dram_tensor(f"scr_{n}_g{ci}", [4 * ns * p, w], BF16).ap()
    xs = [nc.dram_tensor(f"scr_x{i}", [TOK, D], F32).ap() for i in range(2)]
    groups = [[0, 1, 2, 3], [4, 5, 6, 7]]
    with ExitStack() as es0:
        S = Sched(nc, es0)
        for l in range(depth):
            io = dict(scr)
            for k in ('cos', 'sin', 'mem', 'cm', 'pastm', 'ispm', 'ownfix', 'fg'): io[k] = ext[k]
            io['x'] = ext['x'] if l == 0 else xs[l % 2]
            io['xkey'] = (lambda j, l=l: f'xs{l}_{j}')
            io['xokey'] = (lambda j, l=l: f'xs{l + 1}_{j}')
            io['xout'] = ext['xout'] if l == depth - 1 else xs[(l + 1) % 2]
            io['g'] = ext['g_all'][l:l + 1, :]; io['mg'] = ext['mg_all'][l:l + 1, :]
            io['w_in'] = ext['w_in_all'][l * D:(l + 1) * D, :]; io['wkv'] = ext['wkv_all'][l * D:(l + 1) * D, :]
            io['w_branch'] = ext['w_branch_all'][l * 1536:(l + 1) * 1536, :]; io['w_out'] = ext['w_out_all'][l * D:(l + 1) * D, :]

            def after_kv():
                for n, w, p in GATHER:
                    for ci, (s0, ns) in enumerate(GCHUNKS[n]):
                        S.coll(lambda n=n, ci=ci, s0=s0, ns=ns, p=p: nc.gpsimd.collective_compute(
                            "AllGather", ALU.bypass, replica_groups=groups, ins=[scr[n][s0 * p:(s0 + ns) * p, :]], outs=[scr[f'{n}_g{ci}']]),
                            reads=[f'{n}{j}' for j in range(s0, s0 + ns)], writes=[n + '_g'])
            io['after_kv'] = after_kv
            with ExitStack() as es:
                emit_P(nc, S, es, io, pfx=f"P{l}_")
                S.barrier()
            with ExitStack() as es:
                try:
                    emit_A(nc, S, es, io, l == depth - 1, nrun, pfx=f"A{l}_")
                except _StopEmit:
                    pass
                S.barrier()
        S.finish('sp')
        print('fused nins', S.nins, 'nwait', S.nwait, 'nsem', S.nsem)
    return nc


def _core_tokens(r):
    return (np.arange(NSLOT)[:, None] * 4 + r)[:, :, None] * 128 + np.arange(128)[None, None, :]


def _masks(r):
    t = np.arange(128)[:, None]; s = np.arange(512)[None, :]
    cm = np.where(s <= 128 * r + t, 0.0, NEG).astype(np.float32)
    own = 2 * np.arange(NSLOT)[:, None] + r // 2
    blk = np.arange(32)[None, :]
    pastm = np.where(blk < own, 0.0, -1e30).astype(np.float32)
    ispm = np.where(blk < own, NEG, 0.0).astype(np.float32)
    ownfix = np.where(blk <= own, 0.0, NEG).astype(np.float32)
    rep = lambda a: np.ascontiguousarray(np.broadcast_to(a.reshape(1, -1), (128, a.size)))
    return cm, rep(pastm), rep(ispm), rep(ownfix)


_CACHE = {}


def kernel(x, mem, norm_g, w_in, mem_norm_g, w_mem_kv, w_branch, w_out, final_g):
    x = np.asarray(x, np.float32); mem = np.asarray(mem, np.float32)
    if 'F' not in _CACHE:
        _CACHE['F'] = build_fused()
    ncF = _CACHE['F']
    cores = list(range(NCORES))
    pos = [_core_tokens(c % 4).reshape(-1) for c in cores]
    inv_freq = (np.float32(500000.0) ** (-np.arange(8, dtype=np.float32) / np.float32(8))).astype(np.float32)
    f32c = lambda a: np.ascontiguousarray(np.asarray(a, np.float32))
    shared = {'g_all': f32c(norm_g), 'mg_all': f32c(mem_norm_g), 'w_in_all': f32c(w_in).reshape(DEPTH * D, INC),
              'wkv_all': f32c(w_mem_kv).reshape(DEPTH * D, D), 'w_branch_all': f32c(w_branch).reshape(DEPTH * 1536, D),
              'w_out_all': f32c(w_out).reshape(DEPTH * D, D), 'fg': f32c(final_g)[None, :]}
    in_maps = []
    for c in cores:
        ang = pos[c].astype(np.float32)[:, None] * inv_freq[None, :]
        cm, pastm, ispm, ownfix = _masks(c % 4)
        m = dict(shared)
        m.update({'x': np.ascontiguousarray(x[c // 4][pos[c]]), 'cos': np.cos(ang).astype(np.float32), 'sin': np.sin(ang).astype(np.float32),
                  'mem': np.ascontiguousarray(mem[c // 4]), 'cm': cm, 'pastm': pastm, 'ispm': ispm, 'ownfix': ownfix})
        in_maps.append(m)
    res = run_bass_kernel_spmd(ncF, in_maps, core_ids=cores).results
    out = np.empty((2, SEQ, D), np.float32)
    for c in cores:
        out[c // 4][pos[c]] = np.asarray(res[c]['xout'], np.float32)
    return out
```
